# Optimizing a Trainium2 kernel written in Bass

```python
import jax, jax.numpy as jnp
from jax import lax
import numpy as np

D_MODEL = 1024
BATCH = 2
SEQ = 8192
DEPTH = 4

CHUNK = 64
Q_BLOCK = 128
HEAD_DIM = 64
NORM_EPS = 1e-6

SB_HEADS = D_MODEL // (2 * HEAD_DIM)
SB_WIDTH = SB_HEADS * HEAD_DIM
POOL_WINDOWS = (2, 4, 8, 16)
N_POOL = len(POOL_WINDOWS)
POOL_WIDTH = D_MODEL - SB_WIDTH
POOL_GROUP = POOL_WIDTH // N_POOL
EVEN_IN = 3 * SB_WIDTH + POOL_WIDTH

FOX_HEADS = D_MODEL // (2 * HEAD_DIM)
FOX_WIDTH = FOX_HEADS * HEAD_DIM
FOX_IN = 4 * FOX_WIDTH + FOX_HEADS
RWKV_HEADS = D_MODEL // (2 * HEAD_DIM)
RWKV_WIDTH = RWKV_HEADS * HEAD_DIM
DECAY_LORA = 64
ICLR_LORA = 64
GATE_LORA = 128
RWKV_IN = 3 * RWKV_WIDTH + DECAY_LORA + ICLR_LORA + GATE_LORA
ODD_IN = FOX_IN + RWKV_IN
RWKV_GN_EPS = 64e-5

PEER_HEADS = 8
PEER_NKEYS = 128
PEER_EXPERTS = PEER_NKEYS * PEER_NKEYS
PEER_TOPK = 16
PEER_DKEY = 256
PEER_TOKEN_BLOCK = 128

N_EVEN = (DEPTH + 1) // 2
N_ODD = DEPTH // 2

kernel_name = 'hybrid_sb_pool_fox_rwkv7_peer_encoder'


def rms_norm(x, gain, eps=NORM_EPS):
    xf = x.astype(jnp.float32)
    y = xf * lax.rsqrt(jnp.mean(xf * xf, axis=-1, keepdims=True) + eps)
    return (y * gain.astype(jnp.float32)).astype(x.dtype)


def modulate(h, shift, scale):
    return h * (1.0 + scale[:, None, :]) + shift[:, None, :]


def split_heads(t, n_heads):
    b, s, _ = t.shape
    return t.reshape(b, s, n_heads, -1).transpose(0, 2, 1, 3)


def merge_heads(t):
    b, h, s, d = t.shape
    return t.transpose(0, 2, 1, 3).reshape(b, s, h * d)


def stick_breaking_attention(q, k, v):
    b, h, s, dh = q.shape
    nb = s // Q_BLOCK
    scale = dh ** -0.5
    qb = q.reshape(b, h, nb, Q_BLOCK, dh).transpose(2, 0, 1, 3, 4)
    key_pos = jnp.arange(s)

    def block(args):
        qi, i = args
        q_pos = i * Q_BLOCK + jnp.arange(Q_BLOCK)
        strict = key_pos[None, :] < q_pos[:, None]
        z = jnp.einsum('bhqd,bhkd->bhqk', qi, k).astype(jnp.float32) * scale
        log_beta = jax.nn.log_sigmoid(z)
        log_1m = jnp.where(strict, jax.nn.log_sigmoid(-z), 0.0)
        stick = lax.cumsum(log_1m, axis=3, reverse=True) - log_1m
        w = jnp.where(strict, jnp.exp(log_beta + stick), 0.0)
        return jnp.einsum('bhqk,bhkd->bhqd', w.astype(v.dtype), v)

    out = lax.map(block, (qb, jnp.arange(nb)))
    return out.transpose(1, 2, 0, 3, 4).reshape(b, h, s, dh)


def forgetting_attention(q, k, v, log_f):
    b, h, s, dh = q.shape
    nb = s // Q_BLOCK
    scale = dh ** -0.5
    dcum = jnp.cumsum(log_f.astype(jnp.float32), axis=-1)
    qb = q.reshape(b, h, nb, Q_BLOCK, dh).transpose(2, 0, 1, 3, 4)
    db = dcum.reshape(b, h, nb, Q_BLOCK).transpose(2, 0, 1, 3)
    key_pos = jnp.arange(s)

    def block(args):
        qi, di, i = args
        q_pos = i * Q_BLOCK + jnp.arange(Q_BLOCK)
        logits = (jnp.einsum('bhqd,bhkd->bhqk', qi, k).astype(jnp.float32) * scale
                  + di[..., :, None] - dcum[:, :, None, :])
        logits = jnp.where(key_pos[None, :] <= q_pos[:, None], logits, -jnp.inf)
        p = jax.nn.softmax(logits, axis=-1)
        return jnp.einsum('bhqk,bhkd->bhqd', p.astype(v.dtype), v)

    out = lax.map(block, (qb, db, jnp.arange(nb)))
    return out.transpose(1, 2, 0, 3, 4).reshape(b, h, s, dh)


def multiscale_pool(xp, pool_w, pool_scale):
    b, s, _ = xp.shape
    t = jnp.arange(s)
    chunk_end = jnp.minimum((t // CHUNK + 1) * CHUNK, s)
    xg = xp.reshape(b, s, N_POOL, POOL_GROUP).astype(jnp.float32)
    cs = jnp.pad(jnp.cumsum(xg, axis=1), ((0, 0), (1, 0), (0, 0), (0, 0)))
    outs = []
    for g, w in enumerate(POOL_WINDOWS):
        lo = jnp.maximum(t - w // 2, 0)
        hi = jnp.minimum(t + (w - w // 2), chunk_end)
        window_sum = jnp.take(cs[:, :, g], hi, axis=1) - jnp.take(cs[:, :, g], lo, axis=1)
        count = (hi - lo).astype(jnp.float32)[None, :, None]
        outs.append(window_sum / count - xg[:, :, g])
    pooled = jnp.stack(outs, axis=2)
    y = jnp.einsum('bsgc,gcd->bsgd', pooled, pool_w) * pool_scale.reshape(N_POOL, POOL_GROUP)
    return y.reshape(b, s, POOL_WIDTH)


def rwkv7_recurrence(r, w, k, v, kk, a):
    b, s, h, n = r.shape
    xs = tuple(jnp.moveaxis(t.astype(jnp.float32), 1, 0) for t in (r, w, k, v, kk, a))

    def step(state, inp):
        r_t, w_t, k_t, v_t, kk_t, a_t = inp
        sa = jnp.einsum('bhvk,bhk->bhv', state, kk_t)
        state = (state * w_t[:, :, None, :]
                 - sa[..., None] * (kk_t * a_t)[:, :, None, :]
                 + v_t[..., None] * k_t[:, :, None, :])
        y = jnp.einsum('bhvk,bhk->bhv', state, r_t)
        return state, y

    state0 = jnp.zeros((b, h, n, n), jnp.float32)
    _, ys = lax.scan(step, state0, xs)
    return jnp.moveaxis(ys, 0, 1)


def even_mixer(h, w_in, w_out, pool_w, pool_scale):
    proj = h @ w_in
    q = split_heads(proj[..., :SB_WIDTH], SB_HEADS)
    k = split_heads(proj[..., SB_WIDTH:2 * SB_WIDTH], SB_HEADS)
    v = split_heads(proj[..., 2 * SB_WIDTH:3 * SB_WIDTH], SB_HEADS)
    sb = merge_heads(stick_breaking_attention(q, k, v))
    pool = multiscale_pool(proj[..., 3 * SB_WIDTH:], pool_w, pool_scale)
    return jnp.concatenate([sb, pool.astype(sb.dtype)], axis=-1) @ w_out


def odd_mixer(h, w_in, w_out, fox_qnorm, fox_knorm, fox_fbias, rwkv_mix, rwkv_w0, rwkv_w2,
              rwkv_a0, rwkv_a2, rwkv_g2, rwkv_kk, rwkv_ka, rwkv_rk, rwkv_ln_w, rwkv_ln_b):
    b, s, _ = h.shape
    proj = h @ w_in
    pc, pd = proj[..., :FOX_IN], proj[..., FOX_IN:]

    q = rms_norm(split_heads(pc[..., :FOX_WIDTH], FOX_HEADS), fox_qnorm)
    k = rms_norm(split_heads(pc[..., FOX_WIDTH:2 * FOX_WIDTH], FOX_HEADS), fox_knorm)
    v = split_heads(pc[..., 2 * FOX_WIDTH:3 * FOX_WIDTH], FOX_HEADS)
    out_gate = pc[..., 3 * FOX_WIDTH:4 * FOX_WIDTH]
    log_f = jax.nn.log_sigmoid((pc[..., 4 * FOX_WIDTH:] + fox_fbias).astype(jnp.float32))
    att = forgetting_attention(q, k, v, log_f.transpose(0, 2, 1))
    fox_out = merge_heads(att) * jax.nn.sigmoid(out_gate)

    prev = jnp.pad(pd, ((0, 0), (1, 0), (0, 0)))[:, :s]
    pd = pd + (prev - pd) * rwkv_mix
    sizes = (RWKV_WIDTH, RWKV_WIDTH, RWKV_WIDTH, DECAY_LORA, ICLR_LORA, GATE_LORA)
    offs = [0]
    for n in sizes:
        offs.append(offs[-1] + n)
    r, kr, vr, xw, xa, xg = [pd[..., offs[i]:offs[i + 1]] for i in range(6)]
    w_log = -jax.nn.softplus(-(rwkv_w0 + jnp.tanh(xw) @ rwkv_w2)) - 0.5
    decay = jnp.exp(-jnp.exp(w_log.astype(jnp.float32)))
    a = jax.nn.sigmoid(rwkv_a0 + xa @ rwkv_a2)
    g = jax.nn.sigmoid(xg) @ rwkv_g2
    kk = (kr * rwkv_kk).reshape(b, s, RWKV_HEADS, HEAD_DIM).astype(jnp.float32)
    kk = kk / jnp.maximum(jnp.sqrt(jnp.sum(kk * kk, axis=-1, keepdims=True)), 1e-12)
    kr = kr * (1.0 + (a - 1.0) * rwkv_ka)
    hs = (b, s, RWKV_HEADS, HEAD_DIM)
    rh, kh, vh = r.reshape(hs), kr.reshape(hs), vr.reshape(hs)
    y = rwkv7_recurrence(rh, decay.reshape(hs), kh, vh, kk, a.reshape(hs))
    mu = jnp.mean(y, axis=-1, keepdims=True)
    var = jnp.mean(jnp.square(y - mu), axis=-1, keepdims=True)
    y = ((y - mu) * lax.rsqrt(var + RWKV_GN_EPS)).reshape(b, s, RWKV_WIDTH) * rwkv_ln_w + rwkv_ln_b
    bonus = jnp.sum(rh * kh * rwkv_rk, axis=-1, keepdims=True) * vh
    rwkv_out = (y + bonus.reshape(b, s, RWKV_WIDTH)) * g

    return jnp.concatenate([fox_out, rwkv_out.astype(fox_out.dtype)], axis=-1) @ w_out


def peer_ffn(h, w_query, sub_k1, sub_k2, expert_u, expert_v):
    b, s, d = h.shape
    n_tok = b * s
    xt = h.reshape(n_tok, d)
    q = (xt @ w_query).reshape(n_tok, PEER_HEADS, 2, PEER_DKEY // 2)
    s1 = jnp.einsum('thd,nd->thn', q[:, :, 0], sub_k1)
    s2 = jnp.einsum('thd,nd->thn', q[:, :, 1], sub_k2)
    v1, i1 = lax.top_k(s1, PEER_TOPK)
    v2, i2 = lax.top_k(s2, PEER_TOPK)
    cand = (v1[..., :, None] + v2[..., None, :]).reshape(n_tok, PEER_HEADS, PEER_TOPK * PEER_TOPK)
    cand_idx = (i1[..., :, None] * PEER_NKEYS + i2[..., None, :]).reshape(n_tok, PEER_HEADS, PEER_TOPK * PEER_TOPK)
    top_s, pos = lax.top_k(cand.astype(jnp.float32), PEER_TOPK)
    idx = jnp.take_along_axis(cand_idx, pos, axis=-1)
    gate = jax.nn.softmax(top_s, axis=-1)
    nb = n_tok // PEER_TOKEN_BLOCK

    def block(args):
        xb, ib, gb = args
        u = expert_u[ib]
        act = jax.nn.gelu(jnp.einsum('thkd,td->thk', u, xb), approximate=False)
        ve = expert_v[ib]
        return jnp.einsum('thk,thkd->td', (gb * act).astype(ve.dtype), ve)

    out = lax.map(block, (xt.reshape(nb, PEER_TOKEN_BLOCK, d),
                          idx.reshape(nb, PEER_TOKEN_BLOCK, PEER_HEADS, PEER_TOPK),
                          gate.reshape(nb, PEER_TOKEN_BLOCK, PEER_HEADS, PEER_TOPK)))
    return out.reshape(b, s, d)


def setup_inputs(seed: int = 0) -> dict:
    key = jax.random.key(seed)
    ks = iter(jax.random.split(key, 40))

    def nrm(shape, scale):
        return jax.random.normal(next(ks), shape, jnp.float32) * scale

    def gain(shape):
        return 1.0 + nrm(shape, 0.02)

    W = RWKV_WIDTH
    return {
        'x': nrm((BATCH, SEQ, D_MODEL), 1.0),
        'c': nrm((BATCH, D_MODEL), 1.0),
        'ada_w': nrm((DEPTH, D_MODEL, 6 * D_MODEL), 0.5 * D_MODEL ** -0.5),
        'ada_b': nrm((DEPTH, 6 * D_MODEL), 0.01),
        'norm_mix': gain((DEPTH, D_MODEL)),
        'norm_ffn': gain((DEPTH, D_MODEL)),
        'ev_w_in': nrm((N_EVEN, D_MODEL, EVEN_IN), D_MODEL ** -0.5),
        'ev_w_out': nrm((N_EVEN, D_MODEL, D_MODEL), D_MODEL ** -0.5),
        'pool_w': nrm((N_EVEN, N_POOL, POOL_GROUP, POOL_GROUP), POOL_GROUP ** -0.5),
        'pool_scale': gain((N_EVEN, POOL_WIDTH)),
        'od_w_in': nrm((N_ODD, D_MODEL, ODD_IN), D_MODEL ** -0.5),
        'od_w_out': nrm((N_ODD, D_MODEL, D_MODEL), D_MODEL ** -0.5),
        'fox_qnorm': gain((N_ODD, HEAD_DIM)),
        'fox_knorm': gain((N_ODD, HEAD_DIM)),
        'fox_fbias': jnp.linspace(1.0, 6.0, FOX_HEADS)[None, :] + nrm((N_ODD, FOX_HEADS), 0.1),
        'rwkv_mix': jax.random.uniform(next(ks), (N_ODD, RWKV_IN), jnp.float32),
        'rwkv_w0': jax.random.uniform(next(ks), (N_ODD, W), jnp.float32, minval=-6.0, maxval=1.0),
        'rwkv_w2': nrm((N_ODD, DECAY_LORA, W), 0.1 * DECAY_LORA ** -0.5),
        'rwkv_a0': nrm((N_ODD, W), 0.1),
        'rwkv_a2': nrm((N_ODD, ICLR_LORA, W), 0.5 * ICLR_LORA ** -0.5),
        'rwkv_g2': nrm((N_ODD, GATE_LORA, W), GATE_LORA ** -0.5),
        'rwkv_kk': 0.85 + nrm((N_ODD, W), 0.02),
        'rwkv_ka': gain((N_ODD, W)),
        'rwkv_rk': nrm((N_ODD, RWKV_HEADS, HEAD_DIM), 0.1),
        'rwkv_ln_w': gain((N_ODD, W)),
        'rwkv_ln_b': nrm((N_ODD, W), 0.01),
        'peer_wq': nrm((DEPTH, D_MODEL, PEER_HEADS * PEER_DKEY), D_MODEL ** -0.5),
        'peer_k1': nrm((DEPTH, PEER_NKEYS, PEER_DKEY // 2), (PEER_DKEY // 2) ** -0.5),
        'peer_k2': nrm((DEPTH, PEER_NKEYS, PEER_DKEY // 2), (PEER_DKEY // 2) ** -0.5),
        'peer_u': nrm((DEPTH, PEER_EXPERTS, D_MODEL), D_MODEL ** -0.5),
        'peer_v': nrm((DEPTH, PEER_EXPERTS, D_MODEL), PEER_HEADS ** -0.5),
        'final_norm': gain((D_MODEL,)),
    }


def reference(x, c, ada_w, ada_b, norm_mix, norm_ffn, ev_w_in, ev_w_out, pool_w, pool_scale,
              od_w_in, od_w_out, fox_qnorm, fox_knorm, fox_fbias, rwkv_mix, rwkv_w0, rwkv_w2,
              rwkv_a0, rwkv_a2, rwkv_g2, rwkv_kk, rwkv_ka, rwkv_rk, rwkv_ln_w, rwkv_ln_b,
              peer_wq, peer_k1, peer_k2, peer_u, peer_v, final_norm):
    c_act = jax.nn.silu(c)
    for layer in range(DEPTH):
        mod = c_act @ ada_w[layer] + ada_b[layer]
        sh1, sc1, g1, sh2, sc2, g2 = jnp.split(mod, 6, axis=-1)
        h = modulate(rms_norm(x, norm_mix[layer]), sh1, sc1)
        j = layer // 2
        if layer % 2 == 0:
            y = even_mixer(h, ev_w_in[j], ev_w_out[j], pool_w[j], pool_scale[j])
        else:
            y = odd_mixer(h, od_w_in[j], od_w_out[j], fox_qnorm[j], fox_knorm[j], fox_fbias[j],
                          rwkv_mix[j], rwkv_w0[j], rwkv_w2[j], rwkv_a0[j], rwkv_a2[j], rwkv_g2[j],
                          rwkv_kk[j], rwkv_ka[j], rwkv_rk[j], rwkv_ln_w[j], rwkv_ln_b[j])
        x = x + g1[:, None, :] * y
        h = modulate(rms_norm(x, norm_ffn[layer]), sh2, sc2)
        x = x + g2[:, None, :] * peer_ffn(h, peer_wq[layer], peer_k1[layer], peer_k2[layer],
                                          peer_u[layer], peer_v[layer])
    return rms_norm(x, final_norm)
```

```python
import contextlib
import numpy as np
import concourse.bass as bass
import concourse.mybir as mybir
from concourse.bass_utils import run_bass_kernel_spmd

F32 = mybir.dt.float32; BF16 = mybir.dt.bfloat16; I32 = mybir.dt.int32; U32 = mybir.dt.uint32
AF = mybir.ActivationFunctionType; ALU = mybir.AluOpType; AX = mybir.AxisListType

SAME_ENGINE_SYNC = True


class Buf:
    __slots__ = ("name", "writer", "readers", "dma_sem", "dma_count")

    def __init__(self, name):
        self.name = name
        self.writer = None
        self.readers = []
        self.dma_sem = None
        self.dma_count = 0


class Op:
    __slots__ = ("eng", "fn", "deps", "is_dma", "sig", "has_dep", "idx", "wbuf")

    def __init__(self, eng, fn, is_dma):
        self.eng = eng
        self.fn = fn
        self.deps = []
        self.is_dma = is_dma
        self.sig = None
        self.has_dep = False
        self.wbuf = None


class Sched:
    ENGS = ("sp", "act", "dve", "pool", "pe")

    def __init__(self, nc):
        self.nc = nc
        self.ops = {e: [] for e in self.ENGS}
        self.nbuf = 0
        self._nosync = False

    def buf(self, name=None):
        self.nbuf += 1
        return Buf(name or f"b{self.nbuf}")

    def bufs(self, n, name="b"):
        return [self.buf(f"{name}{i}") for i in range(n)]

    def _add(self, op, reads, writes):
        deps = []
        for b in reads:
            if b.writer is not None:
                deps.append(("raw", b.writer))
        for b in writes:
            if b.writer is not None:
                deps.append(("waw", b.writer))
            for r in b.readers:
                deps.append(("war", r))
        seen = set()
        for kind, d in deps:
            if d is op or id(d) in seen:
                continue
            if not d.is_dma and d.eng == op.eng and not op.is_dma:
                if not (SAME_ENGINE_SYNC and kind == "raw" and op.eng != "pe") or self._nosync:
                    continue
            if d.is_dma and op.is_dma and kind == "waw":
                continue
            seen.add(id(d))
            op.deps.append(d)
            d.has_dep = True
        for b in reads:
            b.readers.append(op)
        for b in writes:
            b.writer = op
            b.readers = []
        self.ops[op.eng].append(op)

    def op(self, eng, fn, reads=(), writes=(), nosync=False):
        o = Op(eng, fn, False)
        self._nosync = nosync
        self._add(o, list(reads), list(writes))
        self._nosync = False
        return o

    def dma(self, eng, fn, reads=(), writes=()):
        o = Op(eng, fn, True)
        o.wbuf = writes[0]
        self._add(o, list(reads), list(writes))
        return o

    def emit(self, final_wait_bufs=()):
        nc = self.nc
        with contextlib.ExitStack() as es:
            esems = {e: es.enter_context(nc.semaphore(f"sem_{e}")) for e in self.ENGS}
            for e in self.ENGS:
                cnt = 0
                for o in self.ops[e]:
                    if o.is_dma:
                        b = o.wbuf
                        if b.dma_sem is None:
                            b.dma_sem = es.enter_context(nc.semaphore(f"dsem_{b.name}"))
                        b.dma_count += 16
                        o.sig = (b.dma_sem, b.dma_count)
                    elif o.has_dep:
                        cnt += 1
                        o.sig = (esems[e], cnt)
            block = es.enter_context(nc.Block())
            engobj = {"sp": "sync", "act": "scalar", "dve": "vector", "pool": "gpsimd", "pe": "tensor"}

            def make(e):
                def body(eng):
                    waited = {}
                    for o in self.ops[e]:
                        for d in o.deps:
                            sem, val = d.sig
                            k = id(sem)
                            if waited.get(k, 0) >= val:
                                continue
                            waited[k] = val
                            eng.wait_ge(sem, val)
                        ins = o.fn(eng)
                        if o.is_dma:
                            ins.then_inc(o.sig[0], 16)
                        elif o.has_dep:
                            ins.then_inc(o.sig[0], 1)
                    if e == "sp":
                        for b in final_wait_bufs:
                            eng.wait_ge(b.dma_sem, b.dma_count)
                return body

            for e in self.ENGS:
                if self.ops[e] or e == "sp":
                    getattr(block, engobj[e])(make(e))
D = 1024; KC = 8; TOK = 2048; TB = 512; NTB = TOK // TB
EPS = 1e-6


def emit_mod(S, nc, es, T, P, cact, bcact, adaw_dram, adab_dram, ncols, name):
    nj = ncols // 128
    wts = [T(f"{name}_w{i}", [128, KC, 256], F32) for i in range(2)]
    bwts = S.bufs(2, f"{name}_w")
    bt = T(f"{name}_b", [128, nj], F32); bbt = S.buf(f"{name}_b")
    S.dma("sp", lambda e: e.dma_start(out=bt[:], in_=adab_dram), writes=[bbt])
    ps = P(f"{name}_ps", [128, nj], F32); bps = S.buf(f"{name}_ps")
    for g in range(ncols // 256):
        i = g % 2
        for kc in range(KC):
            S.dma("sp", lambda e, kc=kc, i=i, g=g: e.dma_start(out=wts[i][:, kc, :], in_=adaw_dram[kc * 128:(kc + 1) * 128, g * 256:(g + 1) * 256]), writes=[bwts[i]])
        for jj in range(2):
            j = g * 2 + jj
            for kc in range(KC):
                S.op("pe", lambda e, j=j, jj=jj, kc=kc, i=i: e.matmul(ps[:, j:j + 1], lhsT=wts[i][:, kc, jj * 128:(jj + 1) * 128], rhs=cact[:, kc:kc + 1], start=(kc == 0), stop=(kc == KC - 1)),
                     reads=[bwts[i], bcact], writes=[bps])
    mod = T(f"{name}_mod", [128, nj], F32); bmod = S.buf(f"{name}_mod")
    S.op("dve", lambda e: e.tensor_tensor(out=mod[:], in0=ps[:], in1=bt[:], op=ALU.add), reads=[bps, bbt], writes=[bmod])
    return mod, bmod


def emit_norm_block(S, nc, epsc, xt, bxt, ones_bf, bones, Gs, shift, bmodv, sq, bsq, ps_ss, bps_ss, rstd, brstd, tmp, btmp, hT, bhT):
    for kc in range(KC):
        S.op("act", lambda e, kc=kc: e.activation(out=sq[:, kc, :], in_=xt[:, kc, :], func=AF.Square), reads=[bxt], writes=[bsq])
    for kc in range(KC):
        S.op("pe", lambda e, kc=kc: e.matmul(ps_ss[:], lhsT=ones_bf[:], rhs=sq[:, kc, :], start=(kc == 0), stop=(kc == KC - 1)), reads=[bsq, bones], writes=[bps_ss])
    S.op("act", lambda e: e.activation(out=rstd[:], in_=ps_ss[:], func=AF.Sqrt, bias=epsc[:], scale=1.0 / D), reads=[bps_ss], writes=[brstd])
    S.op("dve", lambda e: e.reciprocal(out=rstd[:], in_=rstd[:]), reads=[brstd], writes=[brstd])
    for kc in range(KC):
        S.op("dve", lambda e, kc=kc: e.scalar_tensor_tensor(out=tmp[:, kc, :], in0=xt[:, kc, :], scalar=Gs[:, kc:kc + 1], in1=rstd[:], op0=ALU.mult, op1=ALU.mult),
             reads=[bxt, brstd, bmodv], writes=[btmp])
        S.op("act", lambda e, kc=kc: e.activation(out=hT[:, kc, :], in_=tmp[:, kc, :], func=AF.Identity, bias=shift[:, kc:kc + 1], scale=1.0),
             reads=[btmp, bmodv], writes=[bhT])


def build_stageA(NOUT):
    nc = bass.Bass("TRN2", target_bir_lowering=False)
    NCH = (NOUT + 127) // 128
    xT = nc.dram_tensor("xT", [D, TOK], F32, kind="ExternalInput").ap()
    cvec = nc.dram_tensor("cvec", [128, KC], F32, kind="ExternalInput").ap()
    adaw = nc.dram_tensor("adaw", [D, 2 * D], F32, kind="ExternalInput").ap()
    adab = nc.dram_tensor("adab", [128, 2 * KC], F32, kind="ExternalInput").ap()
    gain = nc.dram_tensor("gain", [128, KC], F32, kind="ExternalInput").ap()
    w_in = nc.dram_tensor("w_in", [D, NOUT], F32, kind="ExternalInput").ap()
    projT = nc.dram_tensor("projT", [NOUT, TOK], F32, kind="ExternalOutput").ap()
    with contextlib.ExitStack() as es:
        T = lambda n, s, d: es.enter_context(nc.sbuf_tensor(n, s, d))
        P = lambda n, s, d: es.enter_context(nc.psum_tensor(n, s, d))
        S = Sched(nc)
        ct = T("ct", [128, KC], F32); bct = S.buf("ct")
        S.dma("sp", lambda e: e.dma_start(out=ct[:], in_=cvec), writes=[bct])
        cact = T("cact", [128, KC], F32); bcact = S.buf("cact")
        S.op("act", lambda e: e.activation(out=cact[:], in_=ct[:], func=AF.Silu), reads=[bct], writes=[bcact])
        mod, bmod = emit_mod(S, nc, es, T, P, cact, bcact, adaw, adab, 2 * D, "m")
        gt = T("gt", [128, KC], F32); bgt = S.buf("gt")
        S.dma("sp", lambda e: e.dma_start(out=gt[:], in_=gain), writes=[bgt])
        Gs = T("Gs", [128, KC], F32); bmodv = S.buf("modv")
        S.op("dve", lambda e: e.scalar_tensor_tensor(out=Gs[:], in0=mod[:, KC:2 * KC], scalar=1.0, in1=gt[:], op0=ALU.add, op1=ALU.mult), reads=[bmod, bgt], writes=[bmodv])
        shift = mod[:, 0:KC]
        wb = T("wb", [128, KC, NOUT], BF16); bwb = S.buf("wb")
        stg = [T(f"stg{i}", [128, NOUT], F32) for i in range(2)]; bstg = S.bufs(2, "stg")
        for kc in range(KC):
            i = kc % 2
            S.dma("sp", lambda e, kc=kc, i=i: e.dma_start(out=stg[i][:], in_=w_in[kc * 128:(kc + 1) * 128, :]), writes=[bstg[i]])
            eng = "dve" if kc % 2 == 0 else "pool"
            S.op(eng, lambda e, kc=kc, i=i: e.tensor_copy(out=wb[:, kc, :], in_=stg[i][:]), reads=[bstg[i]], writes=[bwb])
        ones_bf = T("ones", [128, 128], BF16); bones = S.buf("ones")
        S.op("pool", lambda e: e.memset(ones_bf[:], 1.0), writes=[bones])
        epsc = T("epsc", [128, 1], F32)
        S.op("pool", lambda e: e.memset(epsc[:], EPS), writes=[bones])
        xt = [T(f"xt{i}", [128, KC, TB], F32) for i in range(2)]; bxt = S.bufs(2, "xt")
        sq = T("sq", [128, KC, TB], BF16); bsq = S.buf("sq")
        ps_ss = P("ps_ss", [128, TB], F32); bps_ss = S.buf("ps_ss")
        rstd = T("rstd", [128, TB], F32); brstd = S.buf("rstd")
        tmp = T("tmp", [128, KC, TB], F32); btmp = S.buf("tmp")
        hT = T("hT", [128, KC, TB], BF16); bhT = S.buf("hT")
        pso = [P(f"pso{i}", [128, TB], F32) for i in range(4)]; bpso = S.bufs(4, "pso")
        ot = [T(f"ot{i}", [128, TB], F32) for i in range(4)]; bot = S.bufs(4, "ot")
        bout = S.buf("out")
        for tb in range(NTB):
            i = tb % 2
            for kc in range(KC):
                S.dma("sp", lambda e, kc=kc, i=i, tb=tb: e.dma_start(out=xt[i][:, kc, :], in_=xT[kc * 128:(kc + 1) * 128, tb * TB:(tb + 1) * TB]), writes=[bxt[i]])
            emit_norm_block(S, nc, epsc, xt[i], bxt[i], ones_bf, bones, Gs, shift, bmodv, sq, bsq, ps_ss, bps_ss, rstd, brstd, tmp, btmp, hT, bhT)
            for n in range(NCH):
                r = min(128, NOUT - n * 128)
                j = n % 4
                for kc in range(KC):
                    S.op("pe", lambda e, kc=kc, n=n, r=r, j=j: e.matmul(pso[j][0:r, :], lhsT=wb[:, kc, n * 128:n * 128 + r], rhs=hT[:, kc, :], start=(kc == 0), stop=(kc == KC - 1)),
                         reads=[bwb, bhT], writes=[bpso[j]])
                if n % 2 == 0:
                    S.op("act", lambda e, r=r, j=j: e.activation(out=ot[j][0:r, :], in_=pso[j][0:r, :], func=AF.Copy), reads=[bpso[j]], writes=[bot[j]])
                else:
                    S.op("dve", lambda e, r=r, j=j: e.tensor_copy(out=ot[j][0:r, :], in_=pso[j][0:r, :]), reads=[bpso[j]], writes=[bot[j]])
                S.dma("sp", lambda e, n=n, r=r, j=j, tb=tb: e.dma_start(out=projT[n * 128:n * 128 + r, tb * TB:(tb + 1) * TB], in_=ot[j][0:r, :]), reads=[bot[j]], writes=[bout])
        S.emit(final_wait_bufs=[bout])
    return nc


def colmajor(v):
    return np.ascontiguousarray(v.reshape(-1, 128).T)
SEQ = 8192; QB = 512; NQ = SEQ // QB; NKB = SEQ // 128
NEG = -30000.0


def sb_consts():
    j = np.arange(128)[:, None]; s = np.arange(128)[None, :]
    U = (j > s).astype(np.float32)
    t = np.arange(512)[None, :]
    m01 = np.stack([((np.arange(128)[:, None] + 128 * r) < t).astype(np.float32) for r in range(4)])
    mneg = (1.0 - m01) * NEG
    return U, m01, mneg


def pool_consts(w):
    out = []
    for tile in (0, 1):
        Mc = np.zeros((128, 128), np.float64); Mp = np.zeros((128, 128), np.float64)
        for tl in range(128):
            t = tile * 128 + tl
            lo = max(t - w // 2, 0); hi = min(t + (w - w // 2), (t // 64 + 1) * 64)
            cnt = hi - lo
            for s in range(lo, hi):
                sl = s - tile * 128
                if sl >= 0: Mc[sl, tl] += 1.0 / cnt
                else: Mp[sl + 128, tl] += 1.0 / cnt
            Mc[tl, tl] -= 1.0
        out.append((Mc, Mp))
    return out[0][0].astype(np.float32), out[1][0].astype(np.float32), out[1][1].astype(np.float32)


def build_stageB_even():
    nc = bass.Bass("TRN2", target_bir_lowering=False)
    qT = nc.dram_tensor("qT", [2, 64, SEQ], F32, kind="ExternalInput").ap()
    kT = nc.dram_tensor("kT", [2, 64, SEQ], F32, kind="ExternalInput").ap()
    vv = nc.dram_tensor("v", [SEQ, 128], F32, kind="ExternalInput").ap()
    xp = nc.dram_tensor("xp", [SEQ, 128], F32, kind="ExternalInput").ap()
    Ud = nc.dram_tensor("U", [128, 128], F32, kind="ExternalInput").ap()
    m01d = nc.dram_tensor("m01", [4, 128, QB], F32, kind="ExternalInput").ap()
    mnegd = nc.dram_tensor("mneg", [4, 128, QB], F32, kind="ExternalInput").ap()
    Md = nc.dram_tensor("M", [3, 128, 128], F32, kind="ExternalInput").ap()
    pwd = nc.dram_tensor("pw", [128, 128], F32, kind="ExternalInput").ap()
    pscd = nc.dram_tensor("psc", [128, 1], F32, kind="ExternalInput").ap()
    sbT = nc.dram_tensor("sbT", [128, SEQ], F32, kind="ExternalOutput").ap()
    poolT = nc.dram_tensor("poolT", [128, SEQ], F32, kind="ExternalOutput").ap()
    with contextlib.ExitStack() as es:
        T = lambda n, s, d: es.enter_context(nc.sbuf_tensor(n, s, d))
        P = lambda n, s, d: es.enter_context(nc.psum_tensor(n, s, d))
        S = Sched(nc)
        stgc = T("stgc", [128, QB], F32); bstgc = S.buf("stgc")
        Ub = T("Ub", [128, 128], BF16); bUb = S.buf("Ub")
        S.dma("sp", lambda e: e.dma_start(out=stgc[:, 0:128], in_=Ud), writes=[bstgc])
        S.op("dve", lambda e: e.tensor_copy(out=Ub[:], in_=stgc[:, 0:128]), reads=[bstgc], writes=[bUb])
        onesb = T("onesb", [128, 128], BF16)
        S.op("pool", lambda e: e.memset(onesb[:], 1.0), writes=[bUb])
        m01 = T("m01s", [128, 4, QB], F32); mneg = T("mnegs", [128, 4, QB], F32); bmask = S.buf("mask")
        for r in range(4):
            S.dma("sp", lambda e, r=r: e.dma_start(out=m01[:, r, :], in_=m01d[r]), writes=[bmask])
            S.dma("sp", lambda e, r=r: e.dma_start(out=mneg[:, r, :], in_=mnegd[r]), writes=[bmask])
        qb = [T(f"qb{h}", [64, SEQ], BF16) for h in range(2)]; kb_ = [T(f"kb{h}", [64, SEQ], BF16) for h in range(2)]
        bq = S.bufs(2, "q"); bk = S.bufs(2, "k")
        stg = [T(f"stg{i}", [128, 2048], F32) for i in range(2)]; bstg = S.bufs(2, "stg")
        cnt = 0
        for h in range(2):
            for (src, dst, bb) in ((qT, qb, bq), (kT, kb_, bk)):
                for c4 in range(4):
                    i = cnt % 2; cnt += 1
                    S.dma("sp", lambda e, i=i, src=src, h=h, c4=c4: e.dma_start(out=stg[i][0:64, :], in_=src[h, :, c4 * 2048:(c4 + 1) * 2048]), writes=[bstg[i]])
                    S.op("dve" if i == 0 else "pool", lambda e, i=i, dst=dst, h=h, c4=c4: e.tensor_copy(out=dst[h][:, c4 * 2048:(c4 + 1) * 2048], in_=stg[i][0:64, :]), reads=[bstg[i]], writes=[bb[h]])
        vb = T("vb", [128, NKB, 128], BF16); bv = S.buf("v")
        vview = vv.rearrange("(n p) c -> p n c", p=128)
        for c4 in range(4):
            i = cnt % 2; cnt += 1
            S.dma("sp", lambda e, i=i, c4=c4: e.dma_start(out=stg[i][:].rearrange("p (n c) -> p n c", c=128), in_=vview[:, c4 * 16:(c4 + 1) * 16, :]), writes=[bstg[i]])
            S.op("dve" if i == 0 else "pool", lambda e, i=i, c4=c4: e.tensor_copy(out=vb[:, c4 * 16:(c4 + 1) * 16, :], in_=stg[i][:].rearrange("p (n c) -> p n c", c=128)), reads=[bstg[i]], writes=[bv])
        ps_z = [P(f"ps_z{i}", [128, QB], F32) for i in range(2)]; bps_z = S.bufs(2, "ps_z")
        ps_st = [P(f"ps_st{i}", [128, QB], F32) for i in range(2)]; bps_st = S.bufs(2, "ps_st")
        ps_o = [P(f"ps_o{i}", [128, QB], F32) for i in range(2)]; bps_o = S.bufs(2, "ps_o")
        NB = 2
        et = [T(f"et{i}", [128, QB], F32) for i in range(NB)]; bet = S.bufs(NB, "et")
        spt = [T(f"spt{i}", [128, QB], F32) for i in range(NB)]; bspt = S.bufs(NB, "spt")
        Lb = [T(f"Lb{i}", [128, QB], BF16) for i in range(NB)]; bLb = S.bufs(NB, "Lb")
        lw = [T(f"lw{i}", [128, QB], F32) for i in range(NB)]; blw = S.bufs(NB, "lw")
        wb = [T(f"wb{i}", [128, QB], BF16) for i in range(NB)]; bwb = S.bufs(NB, "wb")
        Ab = T("Ab", [128, QB], BF16); bAb = S.buf("Ab")
        osb = [T(f"osb{i}", [64, QB], F32) for i in range(2)]; bosb = S.bufs(2, "osb")
        bout = S.buf("out")
        it = 0; sbi = 0
        for h in range(2):
            for Q in range(NQ):
                po = sbi % 2; sbi += 1
                kbs = list(range(4 * Q + 3, -1, -1))
                for n, kb in enumerate(kbs):
                    i = it % NB; z = it % 2; it += 1
                    r = kb - 4 * Q
                    first = (n == 0); last = (n == len(kbs) - 1)
                    S.op("pe", lambda e, z=z, h=h, kb=kb, Q=Q: e.matmul(ps_z[z][:], lhsT=kb_[h][:, kb * 128:(kb + 1) * 128], rhs=qb[h][:, Q * QB:(Q + 1) * QB], start=True, stop=True),
                         reads=[bk[h], bq[h]], writes=[bps_z[z]])
                    S.op("act", lambda e, z=z, i=i: e.activation(out=et[i][:], in_=ps_z[z][:], func=AF.Exp, scale=0.125), reads=[bps_z[z]], writes=[bet[i]])
                    S.op("act", lambda e, i=i: e.activation(out=spt[i][:], in_=et[i][:], func=AF.Ln, bias=1.0, scale=1.0), reads=[bet[i]], writes=[bspt[i]])
                    if r >= 0:
                        S.op("dve", lambda e, i=i, r=r: e.scalar_tensor_tensor(out=Lb[i][:], in0=spt[i][:], scalar=-1.0, in1=m01[:, r, :], op0=ALU.mult, op1=ALU.mult), reads=[bspt[i], bmask], writes=[bLb[i]])
                    else:
                        S.op("dve", lambda e, i=i: e.tensor_scalar(out=Lb[i][:], in0=spt[i][:], scalar1=-1.0, scalar2=None, op0=ALU.mult), reads=[bspt[i]], writes=[bLb[i]])
                    S.op("pe", lambda e, z=z, i=i, first=first: e.matmul(ps_st[z][:], lhsT=Ub[:], rhs=Lb[i][:], start=True, stop=first), reads=[bUb, bLb[i]], writes=[bps_st[z]])
                    if not first:
                        S.op("pe", lambda e, z=z: e.matmul(ps_st[z][:], lhsT=onesb[:], rhs=Ab[:], start=False, stop=True), reads=[bUb, bAb], writes=[bps_st[z]])
                    S.op("dve", lambda e, z=z, i=i: e.scalar_tensor_tensor(out=lw[i][:], in0=ps_z[z][:], scalar=0.125, in1=spt[i][:], op0=ALU.mult, op1=ALU.subtract), reads=[bps_z[z], bspt[i]], writes=[blw[i]])
                    S.op("dve", lambda e, z=z, i=i: e.tensor_tensor(out=lw[i][:], in0=lw[i][:], in1=ps_st[z][:], op=ALU.add), reads=[blw[i], bps_st[z]], writes=[blw[i]])
                    if r >= 0:
                        S.op("pool", lambda e, i=i, r=r: e.tensor_tensor(out=lw[i][:], in0=lw[i][:], in1=mneg[:, r, :], op=ALU.add), reads=[blw[i], bmask], writes=[blw[i]])
                    S.op("act", lambda e, i=i: e.activation(out=wb[i][:], in_=lw[i][:], func=AF.Exp), reads=[blw[i]], writes=[bwb[i]])
                    S.op("pe", lambda e, i=i, po=po, h=h, kb=kb, first=first, last=last: e.matmul(ps_o[po][0:64, :], lhsT=vb[:, kb, h * 64:(h + 1) * 64], rhs=wb[i][:], start=first, stop=last),
                         reads=[bv, bwb[i]], writes=[bps_o[po]])
                    if not last:
                        if first:
                            S.op("pool", lambda e, i=i: e.tensor_copy(out=Ab[:], in_=Lb[i][:]), reads=[bLb[i]], writes=[bAb])
                        else:
                            S.op("pool", lambda e, i=i: e.tensor_tensor(out=Ab[:], in0=Ab[:], in1=Lb[i][:], op=ALU.add), reads=[bLb[i], bAb], writes=[bAb])
                S.op("act", lambda e, po=po: e.activation(out=osb[po][:], in_=ps_o[po][0:64, :], func=AF.Copy), reads=[bps_o[po]], writes=[bosb[po]])
                S.dma("sp", lambda e, po=po, h=h, Q=Q: e.dma_start(out=sbT[h * 64:(h + 1) * 64, Q * QB:(Q + 1) * QB], in_=osb[po][:]), reads=[bosb[po]], writes=[bout])
        xpt = T("xpt", [128, NKB, 128], F32); bxp = S.buf("xp")
        xpv = xp.rearrange("(n p) c -> p n c", p=128)
        for c4 in range(4):
            S.dma("sp", lambda e, c4=c4: e.dma_start(out=xpt[:, c4 * 16:(c4 + 1) * 16, :], in_=xpv[:, c4 * 16:(c4 + 1) * 16, :]), writes=[bxp])
        Mt = T("Mt", [128, 3, 128], F32); pwt = T("pwt", [128, 128], F32); psc = T("pscs", [128, 1], F32); bpc = S.buf("pc")
        for m in range(3):
            S.dma("sp", lambda e, m=m: e.dma_start(out=Mt[:, m, :], in_=Md[m]), writes=[bpc])
        S.dma("sp", lambda e: e.dma_start(out=pwt[:], in_=pwd), writes=[bpc])
        S.dma("sp", lambda e: e.dma_start(out=psc[:], in_=pscd), writes=[bpc])
        pT = [T(f"pT{i}", [128, QB], F32) for i in range(2)]; bpT = S.bufs(2, "pT")
        yT = [T(f"yT{i}", [128, QB], F32) for i in range(2)]; byT = S.bufs(2, "yT")
        bout2 = S.buf("out2")
        for Q in range(NQ):
            z = Q % 2
            for tt in range(4):
                ti = Q * 4 + tt
                S.op("pe", lambda e, z=z, tt=tt, ti=ti: e.matmul(ps_z[z][:, tt * 128:(tt + 1) * 128], lhsT=xpt[:, ti, :], rhs=Mt[:, 0 if ti == 0 else 1, :], start=True, stop=(ti == 0)),
                     reads=[bxp, bpc], writes=[bps_z[z]])
                if ti > 0:
                    S.op("pe", lambda e, z=z, tt=tt, ti=ti: e.matmul(ps_z[z][:, tt * 128:(tt + 1) * 128], lhsT=xpt[:, ti - 1, :], rhs=Mt[:, 2, :], start=False, stop=True),
                         reads=[bxp, bpc], writes=[bps_z[z]])
            S.op("dve", lambda e, z=z: e.tensor_copy(out=pT[z][:], in_=ps_z[z][:]), reads=[bps_z[z]], writes=[bpT[z]])
            S.op("pe", lambda e, z=z: e.matmul(ps_st[z][:], lhsT=pwt[:], rhs=pT[z][:], start=True, stop=True), reads=[bpT[z], bpc], writes=[bps_st[z]])
            S.op("act", lambda e, z=z: e.activation(out=yT[z][:], in_=ps_st[z][:], func=AF.Copy, scale=psc[:, 0:1]), reads=[bps_st[z], bpc], writes=[byT[z]])
            S.dma("sp", lambda e, z=z, Q=Q: e.dma_start(out=poolT[:, Q * QB:(Q + 1) * QB], in_=yT[z][:]), reads=[byT[z]], writes=[bout2])
        S.emit(final_wait_bufs=[bout, bout2])
    return nc
SEQ = 8192; QB = 512; NQ = SEQ // QB; NKB = SEQ // 128
NEG = -30000.0


def fox_consts():
    t = np.arange(512)[None, :]
    mneg = np.stack([(1.0 - ((np.arange(128)[:, None] + 128 * r) <= t).astype(np.float32)) * NEG for r in range(4)]).astype(np.float32)
    sel = np.zeros((65, 64), np.float32); sel[64, :] = 1.0
    return mneg, sel


def build_fox():
    nc = bass.Bass("TRN2", target_bir_lowering=False)
    qT = nc.dram_tensor("qT", [2, 64, SEQ], F32, kind="ExternalInput").ap()
    kT = nc.dram_tensor("kT", [2, 64, SEQ], F32, kind="ExternalInput").ap()
    vv = nc.dram_tensor("v", [SEQ, 128], F32, kind="ExternalInput").ap()
    gT = nc.dram_tensor("gT", [2, 64, SEQ], F32, kind="ExternalInput").ap()
    fl = nc.dram_tensor("fl", [2, SEQ], F32, kind="ExternalInput").ap()
    fb = nc.dram_tensor("fb", [2, 1], F32, kind="ExternalInput").ap()
    qn = nc.dram_tensor("qn", [64, 1], F32, kind="ExternalInput").ap()
    kn = nc.dram_tensor("kn", [64, 1], F32, kind="ExternalInput").ap()
    mnegd = nc.dram_tensor("mneg", [4, 128, QB], F32, kind="ExternalInput").ap()
    seld = nc.dram_tensor("sel", [65, 64], F32, kind="ExternalInput").ap()
    scr = nc.dram_tensor("scr", [2, SEQ], F32, kind="ExternalOutput").ap()
    foxT = nc.dram_tensor("foxT", [128, SEQ], F32, kind="ExternalOutput").ap()
    with contextlib.ExitStack() as es:
        T = lambda n, s, d: es.enter_context(nc.sbuf_tensor(n, s, d))
        P = lambda n, s, d: es.enter_context(nc.psum_tensor(n, s, d))
        S = Sched(nc)
        NDT = T("NDT", [128, SEQ], F32); bNDT = S.buf("NDT")
        stg = [T(f"stg{i}", [128, 2048], F32) for i in range(2)]; bstg = S.bufs(2, "stg")
        flt = NDT[0:2, :]; bfl = bNDT
        S.dma("sp", lambda e: e.dma_start(out=flt, in_=fl), writes=[bfl])
        fbt = T("fbt", [2, 1], F32); bfb = S.buf("fb")
        S.dma("sp", lambda e: e.dma_start(out=fbt[:], in_=fb), writes=[bfb])
        S.op("dve", lambda e: e.tensor_scalar(out=fbt[:], in0=fbt[:], scalar1=-1.0, scalar2=None, op0=ALU.mult), reads=[bfb], writes=[bfb])
        S.op("act", lambda e: e.activation(out=flt, in_=flt, func=AF.Exp, bias=fbt[:], scale=-1.0), reads=[bfl, bfb], writes=[bfl])
        S.op("act", lambda e: e.activation(out=flt, in_=flt, func=AF.Ln, bias=1.0, scale=1.0), reads=[bfl], writes=[bfl])
        onesr = stg[1][0:2, :]; bonesr = bstg[1]
        S.op("pool", lambda e: e.memset(onesr, 1.0), writes=[bonesr])
        ndc = T("ndc", [2, SEQ], F32); bndc = S.buf("ndc")
        for c4 in range(4):
            cs = slice(c4 * 2048, (c4 + 1) * 2048)
            S.op("dve", lambda e, c4=c4, cs=cs: e.tensor_tensor_scan(out=ndc[:, cs], data0=onesr, data1=flt[:, cs], initial=(0.0 if c4 == 0 else ndc[:, c4 * 2048 - 1:c4 * 2048]), op0=ALU.mult, op1=ALU.add),
                 reads=[bfl, bonesr, bndc], writes=[bndc])
        bscr = S.buf("scr")
        S.dma("sp", lambda e: e.dma_start(out=scr, in_=ndc[:]), reads=[bndc], writes=[bscr])
        mneg = T("mnegs", [128, 4, QB], F32); bcst = S.buf("cst")
        for r in range(4):
            S.dma("sp", lambda e, r=r: e.dma_start(out=mneg[:, r, :], in_=mnegd[r]), writes=[bcst])
        sel = T("selt", [65, 64], F32); qnt = T("qnt", [64, 1], F32); knt = T("knt", [64, 1], F32)
        S.dma("sp", lambda e: e.dma_start(out=sel[:], in_=seld), writes=[bcst])
        S.dma("sp", lambda e: e.dma_start(out=qnt[:], in_=qn), writes=[bcst])
        S.dma("sp", lambda e: e.dma_start(out=knt[:], in_=kn), writes=[bcst])
        ones_bf = T("ones", [64, 64], BF16); epsc = T("epsc", [64, 1], F32)
        S.op("pool", lambda e: e.memset(ones_bf[:], 1.0), writes=[bcst])
        S.op("pool", lambda e: e.memset(epsc[:], 1e-6), writes=[bcst])
        vaug = T("vaug", [128, NKB, 2, 65], BF16); bv = S.buf("v")
        S.op("pool", lambda e: e.memset(vaug[:, :, :, 64:65], 1.0), writes=[bv])
        vview = vv.rearrange("(n p) c -> p n c", p=128)
        cnt = 0
        for c4 in range(4):
            i = cnt % 2; cnt += 1
            S.dma("sp", lambda e, i=i, c4=c4: e.dma_start(out=stg[i][:].rearrange("p (n c) -> p n c", c=128), in_=vview[:, c4 * 16:(c4 + 1) * 16, :]), writes=[bstg[i]])
            S.op("dve", lambda e, i=i, c4=c4: e.tensor_copy(out=vaug[:, c4 * 16:(c4 + 1) * 16, :, 0:64], in_=stg[i][:].rearrange("p (n h c) -> p n h c", h=2, c=64)), reads=[bstg[i]], writes=[bv])
        ps_z = [P(f"ps_z{i}", [128, QB], F32) for i in range(2)]; bps_z = S.bufs(2, "ps_z")
        ps_o = [P(f"ps_o{i}", [128, QB], F32) for i in range(2)]; bps_o = S.bufs(2, "ps_o")
        ps_n = P("ps_n", [128, QB], F32); bps_n = S.buf("ps_n")
        ps_d = P("ps_d", [128, QB], F32); bps_d = S.buf("ps_d")
        qnb = T("qnb", [64, SEQ], BF16); knb = T("knb", [64, SEQ], BF16); bqk = [S.buf("qn"), S.buf("kn")]
        sqb = T("sqb", [64, QB], BF16); bsqb = S.buf("sqb")
        rs = T("rs", [64, QB], F32); brs = S.buf("rs")
        ndcol = T("ndcol", [128, NKB], F32); bndcol = S.buf("ndcol")
        NB = 3
        tmp = [T(f"tmp{i}", [128, QB], F32) for i in range(NB)]; btmp = S.bufs(NB, "tmp")
        pb = [T(f"pb{i}", [128, QB], BF16) for i in range(NB)]; bpb = S.bufs(NB, "pb")
        o65 = T("o65", [65, QB], F32); bo65 = S.buf("o65")
        rden = T("rden", [64, QB], F32); brden = S.buf("rden")
        gst = T("gst", [64, QB], F32); bgst = S.buf("gst")
        ot = [T(f"ot{i}", [64, QB], F32) for i in range(2)]; bot = S.bufs(2, "ot")
        bout = S.buf("out")
        it = 0; sbi = 0
        for h in range(2):
            for (src, dst, gcol, bb) in ((qT, qnb, qnt, bqk[0]), (kT, knb, knt, bqk[1])):
                for c4 in range(4):
                    i = cnt % 2; cnt += 1
                    S.dma("sp", lambda e, i=i, src=src, h=h, c4=c4: e.dma_start(out=stg[i][0:64, :], in_=src[h, :, c4 * 2048:(c4 + 1) * 2048]), writes=[bstg[i]])
                    for c in range(4):
                        cs = slice(c * QB, (c + 1) * QB); ds = slice(c4 * 2048 + c * QB, c4 * 2048 + (c + 1) * QB)
                        S.op("act", lambda e, i=i, cs=cs: e.activation(out=sqb[:], in_=stg[i][0:64, cs], func=AF.Square), reads=[bstg[i]], writes=[bsqb])
                        S.op("pe", lambda e: e.matmul(ps_n[0:64, :], lhsT=ones_bf[:], rhs=sqb[:], start=True, stop=True), reads=[bsqb, bcst], writes=[bps_n])
                        S.op("act", lambda e: e.activation(out=rs[:], in_=ps_n[0:64, :], func=AF.Sqrt, bias=epsc[:], scale=1.0 / 64), reads=[bps_n, bcst], writes=[brs])
                        S.op("dve", lambda e: e.reciprocal(out=rs[:], in_=rs[:]), reads=[brs], writes=[brs])
                        S.op("dve", lambda e, i=i, cs=cs, ds=ds, dst=dst, gcol=gcol: e.scalar_tensor_tensor(out=dst[:, ds], in0=stg[i][0:64, cs], scalar=gcol[:, 0:1], in1=rs[:], op0=ALU.mult, op1=ALU.mult),
                             reads=[bstg[i], brs, bcst], writes=[bb])
            S.dma("sp", lambda e, h=h: e.dma_start(out=NDT[:], in_=scr[h:h + 1, :].broadcast_to([128, SEQ])), reads=[bscr], writes=[bNDT])
            S.dma("sp", lambda e, h=h: e.dma_start(out=ndcol[:], in_=scr[h].rearrange("(kb s) -> s kb", s=128), allow_slow_non_contiguous=True), reads=[bscr], writes=[bndcol])
            for Q in range(NQ):
                po = sbi % 2; sbi += 1
                nkb = 4 * Q + 4
                for kb in range(nkb):
                    i = it % NB; z = it % 2; it += 1
                    r = kb - 4 * Q
                    S.op("pe", lambda e, z=z, kb=kb, Q=Q: e.matmul(ps_z[z][:], lhsT=knb[:, kb * 128:(kb + 1) * 128], rhs=qnb[:, Q * QB:(Q + 1) * QB], start=True, stop=True),
                         reads=bqk, writes=[bps_z[z]])
                    S.op("dve", lambda e, z=z, i=i, Q=Q: e.scalar_tensor_tensor(out=tmp[i][:], in0=ps_z[z][:], scalar=0.125, in1=NDT[:, Q * QB:(Q + 1) * QB], op0=ALU.mult, op1=ALU.subtract),
                         reads=[bps_z[z], bNDT], writes=[btmp[i]])
                    if r >= 0:
                        S.op("pool", lambda e, i=i, r=r: e.tensor_tensor(out=tmp[i][:], in0=tmp[i][:], in1=mneg[:, r, :], op=ALU.add), reads=[btmp[i], bcst], writes=[btmp[i]])
                    S.op("act", lambda e, i=i, kb=kb: e.activation(out=pb[i][:], in_=tmp[i][:], func=AF.Exp, bias=ndcol[:, kb:kb + 1], scale=1.0), reads=[btmp[i], bndcol], writes=[bpb[i]])
                    S.op("pe", lambda e, i=i, po=po, h=h, kb=kb, nkb=nkb: e.matmul(ps_o[po][0:65, :], lhsT=vaug[:, kb, h, :], rhs=pb[i][:], start=(kb == 0), stop=(kb == nkb - 1)),
                         reads=[bv, bpb[i]], writes=[bps_o[po]])
                oi = Q % 2
                S.op("act", lambda e, po=po: e.activation(out=o65[:], in_=ps_o[po][0:65, :], func=AF.Copy), reads=[bps_o[po]], writes=[bo65])
                S.op("pe", lambda e: e.matmul(ps_d[0:64, :], lhsT=sel[:], rhs=o65[:], start=True, stop=True), reads=[bo65, bcst], writes=[bps_d])
                S.op("dve", lambda e: e.reciprocal(out=rden[:], in_=ps_d[0:64, :]), reads=[bps_d], writes=[brden])
                S.dma("sp", lambda e, h=h, Q=Q: e.dma_start(out=gst[:], in_=gT[h, :, Q * QB:(Q + 1) * QB]), writes=[bgst])
                S.op("act", lambda e: e.activation(out=gst[:], in_=gst[:], func=AF.Sigmoid), reads=[bgst], writes=[bgst])
                S.op("dve", lambda e: e.tensor_tensor(out=rden[:], in0=rden[:], in1=gst[:], op=ALU.mult), reads=[brden, bgst], writes=[brden])
                S.op("dve", lambda e, oi=oi: e.tensor_tensor(out=ot[oi][:], in0=o65[0:64, :], in1=rden[:], op=ALU.mult), reads=[bo65, brden], writes=[bot[oi]])
                S.dma("sp", lambda e, oi=oi, h=h, Q=Q: e.dma_start(out=foxT[h * 64:(h + 1) * 64, Q * QB:(Q + 1) * QB], in_=ot[oi][:]), reads=[bot[oi]], writes=[bout])
        S.emit(final_wait_bufs=[bout, bscr])
    return nc
SEQ = 8192; RTB = 512; RNTB = SEQ // RTB
CH = 16


def rwkv_consts():
    bd = np.zeros((128, 128), np.float32); bd[:64, :64] = 1; bd[64:, 64:] = 1
    sel2 = np.zeros((2, 128), np.float32); sel2[0, :64] = 1; sel2[1, 64:] = 1
    return bd, np.eye(128, dtype=np.float32), sel2


def build_rwkv(nsteps=SEQ):
    nc = bass.Bass("TRN2", target_bir_lowering=False)
    din = {}
    for n, p in (("rT", 128), ("kT", 128), ("vT", 128), ("xw", 64), ("xa", 64), ("xg", 128)):
        din[n] = nc.dram_tensor(n, [p, SEQ], F32, kind="ExternalInput").ap()
    prmd = nc.dram_tensor("prm", [128, 16], F32, kind="ExternalInput").ap()
    w2d = nc.dram_tensor("w2", [64, 128], F32, kind="ExternalInput").ap()
    a2d = nc.dram_tensor("a2", [64, 128], F32, kind="ExternalInput").ap()
    g2d = nc.dram_tensor("g2", [128, 128], F32, kind="ExternalInput").ap()
    bdd = nc.dram_tensor("bd", [128, 128], F32, kind="ExternalInput").ap()
    identd = nc.dram_tensor("ident", [128, 128], F32, kind="ExternalInput").ap()
    sel2d = nc.dram_tensor("sel2", [2, 128], F32, kind="ExternalInput").ap()
    Xs = nc.dram_tensor("Xs", [2, SEQ, 5, 64], F32, kind="ExternalOutput").ap()
    gsc = nc.dram_tensor("gsc", [128, SEQ], F32, kind="ExternalOutput").ap()
    bgsc = nc.dram_tensor("bgsc", [128, SEQ], F32, kind="ExternalOutput").ap()
    outT = nc.dram_tensor("rwkvT", [128, SEQ], F32, kind="ExternalOutput").ap()
    with contextlib.ExitStack() as es:
        T = lambda n, s, d: es.enter_context(nc.sbuf_tensor(n, s, d))
        P = lambda n, s, d: es.enter_context(nc.psum_tensor(n, s, d))
        S = Sched(nc)
        psx = [P(f"psx{i}", [128, 512], F32) for i in range(8)]; bpsx = S.bufs(8, "psx")
        prm = T("prmt", [128, 16], F32); w2t = T("w2t", [64, 128], F32); a2t = T("a2t", [64, 128], F32); g2t = T("g2t", [128, 128], F32)
        bd = T("bdt", [128, 128], F32); ident = T("identt", [128, 128], F32); sel2 = T("sel2t", [2, 128], F32); bcst = S.buf("cst")
        for (dst, src) in ((prm, prmd), (w2t, w2d), (a2t, a2d), (g2t, g2d), (bd, bdd), (ident, identd), (sel2, sel2d)):
            S.dma("sp", lambda e, dst=dst, src=src: e.dma_start(out=dst[:], in_=src), writes=[bcst])
        omk = T("omk", [128, 1], F32); gneps = T("gneps", [128, 1], F32)
        S.op("dve", lambda e: e.tensor_scalar(out=omk[:], in0=prm[:, 7:8], scalar1=-1.0, scalar2=1.0, op0=ALU.mult, op1=ALU.add), reads=[bcst], writes=[bcst])
        S.op("pool", lambda e: e.memset(gneps[:], 64e-5), writes=[bcst])
        MIXC = {"rT": 0, "kT": 1, "vT": 2, "xg": 3, "xw": 11, "xa": 12}
        vT_all = T("vT_all", [128, SEQ], F32); bvT = S.buf("vT_all")
        yT_all = T("yT_all", [128, SEQ], F32); byT = S.buf("yT_all")
        tin = {n: T(f"in_{n}", [128, RTB + 1], F32) for n in din}; btin = {n: S.buf(f"in_{n}") for n in din}
        tl = {n: T(f"l_{n}", [128, RTB], F32) for n in din if n != "vT"}; btl = {n: S.buf(f"l_{n}") for n in tl}
        names = ("dd", "txw", "sg", "dec", "aa", "sxg", "gt", "kkraw", "sq", "nrm", "kk", "nb", "t1", "kmod", "rkp", "bon")
        W = {n: T(f"w_{n}", [128, RTB], F32) for n in names}; bW = {n: S.buf(f"w_{n}") for n in names}
        xtok = [T(f"xtok{i}", [128, 4, 128], F32) for i in range(2)]; bxtok = S.bufs(2, "xtok")
        bXs = S.buf("Xs"); bgsc_ = S.buf("gsc"); bbgsc = S.buf("bgsc")
        xi_ = 0
        for tb in range(RNTB):
            t0 = tb * RTB
            for n in din:
                p = din[n].shape[0]
                if tb == 0:
                    S.op("pool", lambda e, n=n, p=p: e.memset(tin[n][0:p, 0:1], 0.0), writes=[btin[n]])
                    S.dma("sp", lambda e, n=n, p=p: e.dma_start(out=tin[n][0:p, 1:RTB + 1], in_=din[n][:, 0:RTB]), writes=[btin[n]])
                else:
                    S.dma("sp", lambda e, n=n, p=p, t0=t0: e.dma_start(out=tin[n][0:p, 0:RTB + 1], in_=din[n][:, t0 - 1:t0 + RTB]), writes=[btin[n]])
                dst = vT_all[:, t0:t0 + RTB] if n == "vT" else tl[n][0:p, :]
                bdst = bvT if n == "vT" else btl[n]
                mc = MIXC[n]
                S.op("dve", lambda e, n=n, p=p: e.tensor_tensor(out=W["dd"][0:p, :], in0=tin[n][0:p, 0:RTB], in1=tin[n][0:p, 1:RTB + 1], op=ALU.subtract), reads=[btin[n]], writes=[bW["dd"]])
                S.op("dve", lambda e, n=n, p=p, dst=dst, mc=mc: e.scalar_tensor_tensor(out=dst, in0=W["dd"][0:p, :], scalar=prm[0:p, mc:mc + 1], in1=tin[n][0:p, 1:RTB + 1], op0=ALU.mult, op1=ALU.add),
                     reads=[bW["dd"], btin[n], bcst], writes=[bdst])
            rl, kl, xwl, xal, xgl = tl["rT"], tl["kT"], tl["xw"], tl["xa"], tl["xg"]
            vl = vT_all[:, t0:t0 + RTB]
            S.op("act", lambda e: e.activation(out=W["txw"][0:64, :], in_=xwl[0:64, :], func=AF.Tanh), reads=[btl["xw"]], writes=[bW["txw"]])
            S.op("pe", lambda e: e.matmul(psx[0][:], lhsT=w2t[:], rhs=W["txw"][0:64, :], start=True, stop=True), reads=[bW["txw"], bcst], writes=[bpsx[0]])
            S.op("act", lambda e: e.activation(out=W["sg"][:], in_=psx[0][:], func=AF.Sigmoid, bias=prm[:, 4:5], scale=1.0), reads=[bpsx[0], bcst], writes=[bW["sg"]])
            S.op("act", lambda e: e.activation(out=W["dec"][:], in_=W["sg"][:], func=AF.Exp, scale=-0.6065306597126334), reads=[bW["sg"]], writes=[bW["dec"]])
            S.op("pe", lambda e: e.matmul(psx[1][:], lhsT=a2t[:], rhs=xal[0:64, :], start=True, stop=True), reads=[btl["xa"], bcst], writes=[bpsx[1]])
            S.op("act", lambda e: e.activation(out=W["aa"][:], in_=psx[1][:], func=AF.Sigmoid, bias=prm[:, 5:6], scale=1.0), reads=[bpsx[1], bcst], writes=[bW["aa"]])
            S.op("act", lambda e: e.activation(out=W["sxg"][:], in_=xgl[:], func=AF.Sigmoid), reads=[btl["xg"]], writes=[bW["sxg"]])
            S.op("pe", lambda e: e.matmul(psx[2][:], lhsT=g2t[:], rhs=W["sxg"][:], start=True, stop=True), reads=[bW["sxg"], bcst], writes=[bpsx[2]])
            S.op("act", lambda e: e.activation(out=W["gt"][:], in_=psx[2][:], func=AF.Copy), reads=[bpsx[2]], writes=[bW["gt"]])
            S.dma("sp", lambda e, t0=t0: e.dma_start(out=gsc[:, t0:t0 + RTB], in_=W["gt"][:]), reads=[bW["gt"]], writes=[bgsc_])
            S.op("dve", lambda e: e.tensor_scalar(out=W["kkraw"][:], in0=kl[:], scalar1=prm[:, 6:7], scalar2=None, op0=ALU.mult), reads=[btl["kT"], bcst], writes=[bW["kkraw"]])
            S.op("dve", lambda e: e.tensor_tensor(out=W["sq"][:], in0=W["kkraw"][:], in1=W["kkraw"][:], op=ALU.mult), reads=[bW["kkraw"]], writes=[bW["sq"]])
            S.op("pe", lambda e: e.matmul(psx[3][:], lhsT=bd[:], rhs=W["sq"][:], start=True, stop=True), reads=[bW["sq"], bcst], writes=[bpsx[3]])
            S.op("act", lambda e: e.activation(out=W["nrm"][:], in_=psx[3][:], func=AF.Sqrt), reads=[bpsx[3]], writes=[bW["nrm"]])
            S.op("dve", lambda e: e.tensor_scalar(out=W["nrm"][:], in0=W["nrm"][:], scalar1=1e-12, scalar2=None, op0=ALU.max), reads=[bW["nrm"]], writes=[bW["nrm"]])
            S.op("dve", lambda e: e.reciprocal(out=W["nrm"][:], in_=W["nrm"][:]), reads=[bW["nrm"]], writes=[bW["nrm"]])
            S.op("dve", lambda e: e.tensor_tensor(out=W["kk"][:], in0=W["kkraw"][:], in1=W["nrm"][:], op=ALU.mult), reads=[bW["kkraw"], bW["nrm"]], writes=[bW["kk"]])
            S.op("dve", lambda e: e.scalar_tensor_tensor(out=W["nb"][:], in0=W["kk"][:], scalar=-1.0, in1=W["aa"][:], op0=ALU.mult, op1=ALU.mult), reads=[bW["kk"], bW["aa"]], writes=[bW["nb"]])
            S.op("dve", lambda e: e.tensor_scalar(out=W["t1"][:], in0=W["aa"][:], scalar1=prm[:, 7:8], scalar2=omk[:, 0:1], op0=ALU.mult, op1=ALU.add), reads=[bW["aa"], bcst], writes=[bW["t1"]])
            S.op("dve", lambda e: e.tensor_tensor(out=W["kmod"][:], in0=kl[:], in1=W["t1"][:], op=ALU.mult), reads=[btl["kT"], bW["t1"]], writes=[bW["kmod"]])
            S.op("dve", lambda e: e.scalar_tensor_tensor(out=W["rkp"][:], in0=rl[:], scalar=prm[:, 8:9], in1=W["kmod"][:], op0=ALU.mult, op1=ALU.mult), reads=[btl["rT"], bW["kmod"], bcst], writes=[bW["rkp"]])
            S.op("pe", lambda e: e.matmul(psx[4][:], lhsT=bd[:], rhs=W["rkp"][:], start=True, stop=True), reads=[bW["rkp"], bcst], writes=[bpsx[4]])
            S.op("dve", lambda e, vl=vl: e.tensor_tensor(out=W["bon"][:], in0=psx[4][:], in1=vl, op=ALU.mult), reads=[bpsx[4], bvT], writes=[bW["bon"]])
            S.op("dve", lambda e: e.tensor_tensor(out=W["bon"][:], in0=W["bon"][:], in1=W["gt"][:], op=ALU.mult), reads=[bW["bon"], bW["gt"]], writes=[bW["bon"]])
            S.dma("sp", lambda e, t0=t0: e.dma_start(out=bgsc[:, t0:t0 + RTB], in_=W["bon"][:]), reads=[bW["bon"]], writes=[bbgsc])
            for Xi, (Xt, bX) in enumerate(((W["kk"], bW["kk"]), (W["dec"], bW["dec"]), (W["nb"], bW["nb"]), (W["kmod"], bW["kmod"]), (rl, btl["rT"]))):
                xi = xi_ % 2; xi_ += 1
                for ts in range(4):
                    S.op("pe", lambda e, Xt=Xt, ts=ts: e.transpose(out=psx[5][:, ts * 128:(ts + 1) * 128], in_=Xt[:, ts * 128:(ts + 1) * 128], identity=ident[:]), reads=[bX, bcst], writes=[bpsx[5]])
                S.op("act", lambda e, xi=xi: e.activation(out=xtok[xi][:].rearrange("p a c -> p (a c)"), in_=psx[5][:], func=AF.Copy), reads=[bpsx[5]], writes=[bxtok[xi]])
                for h in range(2):
                    S.dma("sp", lambda e, xi=xi, h=h, Xi=Xi, t0=t0: e.dma_start(out=Xs[h, t0:t0 + RTB, Xi, :].rearrange("(a p) k -> p a k", p=128), in_=xtok[xi][:, :, h * 64:(h + 1) * 64]),
                          reads=[bxtok[xi]], writes=[bXs])
        St = T("St", [128, 64], F32); bS = S.buf("S")
        junk = T("junk", [128, 64], F32); sa = T("sa", [128, 1], F32)
        S.op("dve", lambda e: e.memset(St[:], 0.0), writes=[bS])
        NR = 2
        rows = [T(f"rows{i}", [2, CH, 5, 64], F32) for i in range(NR)]; brows = S.bufs(NR, "rows")
        NG = 3
        opnd = [[T(f"opnd{g}_{x}", [128, 512], F32) for x in range(5)] for g in range(NG)]; bopnd = [S.bufs(5, f"opnd{g}_") for g in range(NG)]
        pr = 0; gi = 0
        for c in range(nsteps // CH):
            ri = c % NR
            S.dma("sp", lambda e, ri=ri, c=c: e.dma_start(out=rows[ri][:], in_=Xs[:, c * CH:(c + 1) * CH, :, :]), reads=[bXs], writes=[brows[ri]])
            for g in range(CH // 8):
                gg = gi % NG; gi += 1
                for x in range(5):
                    pi = pr % 4; pr += 1
                    S.op("pe", lambda e, pi=pi, ri=ri, g=g, x=x: e.matmul(psx[pi][:].rearrange("p (a k) -> p a k", k=64), lhsT=sel2[:], rhs=rows[ri][:, g * 8:(g + 1) * 8, x, :], start=True, stop=True),
                         reads=[brows[ri], bcst], writes=[bpsx[pi]])
                    S.op("act", lambda e, pi=pi, gg=gg, x=x: e.activation(out=opnd[gg][x][:], in_=psx[pi][:], func=AF.Copy), reads=[bpsx[pi]], writes=[bopnd[gg][x]])
                for j in range(8):
                    t = c * CH + g * 8 + j
                    js = slice(j * 64, (j + 1) * 64)
                    kkB, wB, nbB, kB, rB = (opnd[gg][x][:, js] for x in range(5))
                    S.op("dve", lambda e, kkB=kkB: e.scalar_tensor_tensor(out=junk[:], in0=St[:], scalar=1.0, in1=kkB, op0=ALU.mult, op1=ALU.mult, accum_out=sa[:]), reads=[bS, bopnd[gg][0]], writes=[bS], nosync=True)
                    S.op("dve", lambda e, wB=wB: e.tensor_tensor(out=St[:], in0=St[:], in1=wB, op=ALU.mult), reads=[bS, bopnd[gg][1]], writes=[bS], nosync=True)
                    S.op("dve", lambda e, nbB=nbB: e.scalar_tensor_tensor(out=St[:], in0=nbB, scalar=sa[:, 0:1], in1=St[:], op0=ALU.mult, op1=ALU.add), reads=[bS, bopnd[gg][2]], writes=[bS], nosync=True)
                    S.op("dve", lambda e, kB=kB, t=t: e.scalar_tensor_tensor(out=St[:], in0=kB, scalar=vT_all[:, t:t + 1], in1=St[:], op0=ALU.mult, op1=ALU.add), reads=[bS, bopnd[gg][3], bvT], writes=[bS], nosync=True)
                    S.op("dve", lambda e, rB=rB, t=t: e.scalar_tensor_tensor(out=junk[:], in0=St[:], scalar=1.0, in1=rB, op0=ALU.mult, op1=ALU.mult, accum_out=yT_all[:, t:t + 1]), reads=[bS, bopnd[gg][4]], writes=[bS, byT], nosync=True)
        bout = S.buf("out")
        yc = W["dd"]; byc = bW["dd"]; sq = W["sq"]; bsq = bW["sq"]; sd = W["nrm"]; bsd = bW["nrm"]; gl = W["gt"]; bgl = bW["gt"]; bl = W["bon"]; bbl = bW["bon"]; ob = W["kk"]; bob = bW["kk"]
        for tb in range(RNTB):
            t0 = tb * RTB
            if t0 >= nsteps:
                break
            y = yT_all[:, t0:t0 + RTB]
            S.op("pe", lambda e, y=y: e.matmul(psx[4][:], lhsT=bd[:], rhs=y, start=True, stop=True), reads=[byT, bcst], writes=[bpsx[4]])
            S.op("dve", lambda e, y=y: e.scalar_tensor_tensor(out=yc[:], in0=psx[4][:], scalar=-1.0 / 64, in1=y, op0=ALU.mult, op1=ALU.add), reads=[bpsx[4], byT], writes=[byc])
            S.op("dve", lambda e: e.tensor_tensor(out=sq[:], in0=yc[:], in1=yc[:], op=ALU.mult), reads=[byc], writes=[bsq])
            S.op("pe", lambda e: e.matmul(psx[5][:], lhsT=bd[:], rhs=sq[:], start=True, stop=True), reads=[bsq, bcst], writes=[bpsx[5]])
            S.op("act", lambda e: e.activation(out=sd[:], in_=psx[5][:], func=AF.Sqrt, bias=gneps[:], scale=1.0 / 64), reads=[bpsx[5], bcst], writes=[bsd])
            S.op("dve", lambda e: e.reciprocal(out=sd[:], in_=sd[:]), reads=[bsd], writes=[bsd])
            S.op("dve", lambda e: e.tensor_tensor(out=yc[:], in0=yc[:], in1=sd[:], op=ALU.mult), reads=[byc, bsd], writes=[byc])
            S.op("dve", lambda e: e.tensor_scalar(out=yc[:], in0=yc[:], scalar1=prm[:, 9:10], scalar2=prm[:, 10:11], op0=ALU.mult, op1=ALU.add), reads=[byc, bcst], writes=[byc])
            S.dma("sp", lambda e, t0=t0: e.dma_start(out=gl[:], in_=gsc[:, t0:t0 + RTB]), reads=[bgsc_], writes=[bgl])
            S.dma("sp", lambda e, t0=t0: e.dma_start(out=bl[:], in_=bgsc[:, t0:t0 + RTB]), reads=[bbgsc], writes=[bbl])
            S.op("dve", lambda e: e.tensor_tensor(out=ob[:], in0=yc[:], in1=gl[:], op=ALU.mult), reads=[byc, bgl], writes=[bob])
            S.op("dve", lambda e: e.tensor_tensor(out=ob[:], in0=ob[:], in1=bl[:], op=ALU.add), reads=[bob, bbl], writes=[bob])
            S.dma("sp", lambda e, t0=t0: e.dma_start(out=outT[:, t0:t0 + RTB], in_=ob[:]), reads=[bob], writes=[bout])
        S.emit(final_wait_bufs=[bout, bXs, bgsc_, bbgsc])
    return nc
D = 1024; KC = 8; TOK = 2048
EPS = 1e-6
NEXP = 16384


def build_stageC(final):
    TB = 256; NTB = TOK // TB; NTT = TB // 128
    nc = bass.Bass("TRN2", target_bir_lowering=False)
    xT = nc.dram_tensor("xT", [D, TOK], F32, kind="ExternalInput").ap()
    mixT = nc.dram_tensor("mixT", [D, TOK], F32, kind="ExternalInput").ap()
    cvec = nc.dram_tensor("cvec", [128, KC], F32, kind="ExternalInput").ap()
    adaw = nc.dram_tensor("adaw", [D, 4 * D], F32, kind="ExternalInput").ap()
    adab = nc.dram_tensor("adab", [128, 4 * KC], F32, kind="ExternalInput").ap()
    gain = nc.dram_tensor("gain", [128, KC], F32, kind="ExternalInput").ap()
    fgain = nc.dram_tensor("fgain", [128, KC], F32, kind="ExternalInput").ap()
    w_out = nc.dram_tensor("w_out", [D, D], F32, kind="ExternalInput").ap()
    wq = nc.dram_tensor("wq", [D, 2 * D], F32, kind="ExternalInput").ap()
    k12T = nc.dram_tensor("k12T", [128, 256], F32, kind="ExternalInput").ap()
    identd = nc.dram_tensor("ident", [128, 128], F32, kind="ExternalInput").ap()
    pu = nc.dram_tensor("pu", [NEXP, D], F32, kind="ExternalInput").ap()
    pv = nc.dram_tensor("pv", [NEXP, D], F32, kind="ExternalInput").ap()
    outT = nc.dram_tensor("outT", [D, TOK], F32, kind="ExternalOutput").ap()
    with contextlib.ExitStack() as es:
        T = lambda n, s, d: es.enter_context(nc.sbuf_tensor(n, s, d))
        P = lambda n, s, d: es.enter_context(nc.psum_tensor(n, s, d))
        S = Sched(nc)
        ps_misc = P("ps_misc", [128, 512], F32); bps_misc = S.buf("ps_misc")
        ps_ss = P("ps_ss", [128, 512], F32); bps_ss = S.buf("ps_ss")
        pso = [P(f"pso{i}", [128, 512], F32) for i in range(2)]; bpso = S.bufs(2, "pso")
        ps_sc = P("ps_sc", [128, 2048], F32); bps_sc = S.buf("ps_sc")
        ct = T("ct", [128, KC], F32); bct = S.buf("ct")
        S.dma("sp", lambda e: e.dma_start(out=ct[:], in_=cvec), writes=[bct])
        cact = T("cact", [128, KC], F32); bcact = S.buf("cact")
        S.op("act", lambda e: e.activation(out=cact[:], in_=ct[:], func=AF.Silu), reads=[bct], writes=[bcact])
        stg = [T(f"stg{i}", [128, KC, 256], F32) for i in range(2)]; bstg = S.bufs(2, "stg")
        bt = T("adabt", [128, 4 * KC], F32); bbt = S.buf("adabt")
        S.dma("sp", lambda e: e.dma_start(out=bt[:], in_=adab), writes=[bbt])
        for g8 in range(16):
            i = g8 % 2
            for kc in range(KC):
                S.dma("sp", lambda e, kc=kc, i=i, g8=g8: e.dma_start(out=stg[i][:, kc, :], in_=adaw[kc * 128:(kc + 1) * 128, g8 * 256:(g8 + 1) * 256]), writes=[bstg[i]])
            for jj in range(2):
                j = g8 * 2 + jj
                for kc in range(KC):
                    S.op("pe", lambda e, j=j, jj=jj, kc=kc, i=i: e.matmul(ps_misc[:, j:j + 1], lhsT=stg[i][:, kc, jj * 128:(jj + 1) * 128], rhs=cact[:, kc:kc + 1], start=(kc == 0), stop=(kc == KC - 1)),
                         reads=[bstg[i], bcact], writes=[bps_misc])
        mod = T("mod", [128, 4 * KC], F32); bmod = S.buf("mod")
        S.op("dve", lambda e: e.tensor_tensor(out=mod[:], in0=ps_misc[:, 0:4 * KC], in1=bt[:], op=ALU.add), reads=[bps_misc, bbt], writes=[bmod])
        gt = T("gt", [128, KC], F32); fg = T("fg", [128, KC], F32); bgt = S.buf("gt")
        S.dma("sp", lambda e: e.dma_start(out=gt[:], in_=gain), writes=[bgt])
        S.dma("sp", lambda e: e.dma_start(out=fg[:], in_=fgain), writes=[bgt])
        Gs = T("Gs", [128, KC], F32)
        S.op("dve", lambda e: e.scalar_tensor_tensor(out=Gs[:], in0=mod[:, 16:24], scalar=1.0, in1=gt[:], op0=ALU.add, op1=ALU.mult), reads=[bmod, bgt], writes=[bmod])
        g1 = mod[:, 0:8]; sh2 = mod[:, 8:16]; g2 = mod[:, 24:32]
        wob = T("wob", [128, KC, D], BF16); wqb = T("wqb", [128, KC, 2 * D], BF16); bw = S.buf("w")
        cnt = 0
        for (src, dst, ncol) in ((w_out, wob, D), (wq, wqb, 2 * D)):
            for c5 in range(ncol // 256):
                i = cnt % 2; cnt += 1
                for kc in range(KC):
                    S.dma("sp", lambda e, kc=kc, i=i, c5=c5, src=src: e.dma_start(out=stg[i][:, kc, :], in_=src[kc * 128:(kc + 1) * 128, c5 * 256:(c5 + 1) * 256]), writes=[bstg[i]])
                S.op("dve" if i == 0 else "pool", lambda e, i=i, c5=c5, dst=dst: e.tensor_copy(out=dst[:, :, c5 * 256:(c5 + 1) * 256], in_=stg[i][:]), reads=[bstg[i]], writes=[bw])
        kst = T("kst", [128, 256], F32); k12b = T("k12b", [128, 256], BF16); ident = T("identt", [128, 128], F32); bcst = S.buf("cst")
        S.dma("sp", lambda e: e.dma_start(out=kst[:], in_=k12T), writes=[bcst])
        S.dma("sp", lambda e: e.dma_start(out=ident[:], in_=identd), writes=[bcst])
        S.op("dve", lambda e: e.tensor_copy(out=k12b[:], in_=kst[:]), reads=[bcst], writes=[bcst])
        ones_bf = T("ones", [128, 128], BF16); epsc = T("epsc", [128, 1], F32)
        S.op("pool", lambda e: e.memset(ones_bf[:], 1.0), writes=[bcst])
        S.op("pool", lambda e: e.memset(epsc[:], EPS), writes=[bcst])
        mst = [T(f"mst{i}", [128, TB], F32) for i in range(2)]; bmst = S.bufs(2, "mst")
        mixb = T("mixb", [128, KC, TB], BF16); bmixb = S.buf("mixb")
        x1 = T("x1", [128, KC, TB], F32); bx1 = S.buf("x1")
        sq = T("sq", [128, KC, TB], BF16); bsq = S.buf("sq")
        rstd = T("rstd", [128, TB], F32); brstd = S.buf("rstd")
        h2f = T("h2f", [128, KC, TB], F32); bh2f = S.buf("h2f")
        h2b = T("h2b", [128, KC, TB], BF16); bh2b = S.buf("h2b")
        qTb = T("qTb", [128, 16, TB], BF16); bqTb = S.buf("qTb")
        x2 = T("x2", [128, KC, TB], F32); bx2 = S.buf("x2")
        h2tok = T("h2tok", [128, D], F32); bh2tok = S.buf("h2tok")
        sall = T("sall", [128, 16, 128], F32); bsall = S.buf("sall")
        swork = T("swork", [128, 16, 128], F32); bswork = S.buf("swork")
        v12 = T("v12", [128, 16, 16], F32); bv12 = S.buf("v12")
        i12u = T("i12u", [128, 16, 16], U32); i12f = T("i12f", [128, 16, 16], F32); bi12 = S.buf("i12")
        cand = T("cand", [128, 8, 256], F32); bcand = S.buf("cand")
        cwork = T("cwork", [128, 8, 256], F32); bcwork = S.buf("cwork")
        cidx = T("cidx", [128, 8, 256], F32); bcidx = S.buf("cidx")
        tops = T("tops", [128, 8, 16], F32); btops = S.buf("tops")
        eq = T("eq", [128, 8, 256], F32); beq = S.buf("eq")
        idxf = T("idxf", [128, 8, 16], F32); bidxf = S.buf("idxf")
        idxi = T("idxi", [128, 128], I32); bidxi = S.buf("idxi")
        gmx = T("gmx", [128, 8], F32); gsum = T("gsum", [128, 8], F32); gate = T("gate", [128, 8, 16], F32); bgate = S.buf("gate")
        aact = T("aact", [128, 128], F32); baact = S.buf("aact")
        coef = T("coef", [128, 128], F32); bcoef = S.buf("coef")
        junk = T("junk", [128, D], F32); bjunk = S.buf("junk")
        NG = 4
        ug = [T(f"ug{i}", [128, D], F32) for i in range(NG)]; bug = S.bufs(NG, "ug")
        acc = T("acc", [128, D], F32); bacc = S.buf("acc")
        osb, bosb = h2f, bh2f
        bout = S.buf("out")
        gi = 0

        def norm(src, bsrc, GsAP, shiftAP, dstf, bdstf, bvec):
            for kc in range(KC):
                S.op("act", lambda e, kc=kc: e.activation(out=sq[:, kc, :], in_=src[:, kc, :], func=AF.Square), reads=[bsrc], writes=[bsq])
            for kc in range(KC):
                S.op("pe", lambda e, kc=kc: e.matmul(ps_ss[:, 0:TB], lhsT=ones_bf[:], rhs=sq[:, kc, :], start=(kc == 0), stop=(kc == KC - 1)), reads=[bsq, bcst], writes=[bps_ss])
            S.op("act", lambda e: e.activation(out=rstd[:], in_=ps_ss[:, 0:TB], func=AF.Sqrt, bias=epsc[:], scale=1.0 / D), reads=[bps_ss, bcst], writes=[brstd])
            S.op("dve", lambda e: e.reciprocal(out=rstd[:], in_=rstd[:]), reads=[brstd], writes=[brstd])
            for kc in range(KC):
                S.op("dve", lambda e, kc=kc: e.scalar_tensor_tensor(out=dstf[:, kc, :], in0=src[:, kc, :], scalar=GsAP[:, kc:kc + 1], in1=rstd[:], op0=ALU.mult, op1=ALU.mult),
                     reads=[bsrc, brstd, bvec], writes=[bdstf])
                if shiftAP is not None:
                    S.op("act", lambda e, kc=kc: e.activation(out=dstf[:, kc, :], in_=dstf[:, kc, :], func=AF.Identity, bias=shiftAP[:, kc:kc + 1], scale=1.0),
                         reads=[bdstf, bvec], writes=[bdstf])

        for tb in range(NTB):
            t0 = tb * TB
            for kc in range(KC):
                i = kc % 2
                S.dma("sp", lambda e, kc=kc, i=i, t0=t0: e.dma_start(out=mst[i][:], in_=mixT[kc * 128:(kc + 1) * 128, t0:t0 + TB]), writes=[bmst[i]])
                S.op("pool", lambda e, kc=kc, i=i: e.tensor_copy(out=mixb[:, kc, :], in_=mst[i][:]), reads=[bmst[i]], writes=[bmixb])
                S.dma("sp", lambda e, kc=kc, t0=t0: e.dma_start(out=x1[:, kc, :], in_=xT[kc * 128:(kc + 1) * 128, t0:t0 + TB]), writes=[bx1])
            for n in range(KC):
                j = n % 2
                for kc in range(KC):
                    S.op("pe", lambda e, kc=kc, n=n, j=j: e.matmul(pso[j][:, 0:TB], lhsT=wob[:, kc, n * 128:(n + 1) * 128], rhs=mixb[:, kc, :], start=(kc == 0), stop=(kc == KC - 1)),
                         reads=[bw, bmixb], writes=[bpso[j]])
                S.op("dve", lambda e, n=n, j=j: e.scalar_tensor_tensor(out=x1[:, n, :], in0=pso[j][:, 0:TB], scalar=g1[:, n:n + 1], in1=x1[:, n, :], op0=ALU.mult, op1=ALU.add),
                     reads=[bpso[j], bx1, bmod], writes=[bx1])
            norm(x1, bx1, Gs, sh2, h2f, bh2f, bmod)
            S.op("pool", lambda e: e.tensor_copy(out=h2b[:], in_=h2f[:]), reads=[bh2f], writes=[bh2b])
            for n in range(16):
                j = n % 2
                for kc in range(KC):
                    S.op("pe", lambda e, kc=kc, n=n, j=j: e.matmul(pso[j][:, 0:TB], lhsT=wqb[:, kc, n * 128:(n + 1) * 128], rhs=h2b[:, kc, :], start=(kc == 0), stop=(kc == KC - 1)),
                         reads=[bw, bh2b], writes=[bpso[j]])
                S.op("act", lambda e, n=n, j=j: e.activation(out=qTb[:, n, :], in_=pso[j][:, 0:TB], func=AF.Copy), reads=[bpso[j]], writes=[bqTb])
            for tt in range(NTT):
                ts = slice(tt * 128, (tt + 1) * 128)
                for half in range(2):
                    for k4 in range(4):
                        kc = half * 4 + k4
                        S.op("pe", lambda e, kc=kc, k4=k4, ts=ts: e.transpose(out=ps_misc[:, k4 * 128:(k4 + 1) * 128], in_=h2f[:, kc, ts], identity=ident[:]), reads=[bh2f, bcst], writes=[bps_misc])
                    S.op("act", lambda e, half=half: e.activation(out=h2tok[:, half * 512:(half + 1) * 512], in_=ps_misc[:], func=AF.Copy), reads=[bps_misc], writes=[bh2tok])
                for n in range(16):
                    hf = n % 2
                    S.op("pe", lambda e, n=n, hf=hf, ts=ts: e.matmul(ps_sc[:, n * 128:(n + 1) * 128], lhsT=qTb[:, n, ts], rhs=k12b[:, hf * 128:(hf + 1) * 128], start=True, stop=True),
                         reads=[bqTb, bcst], writes=[bps_sc])
                S.op("act", lambda e: e.activation(out=sall[:].rearrange("p a b -> p (a b)"), in_=ps_sc[:], func=AF.Copy), reads=[bps_sc], writes=[bsall])
                for n in range(16):
                    S.op("dve", lambda e, n=n: e.max(out=v12[:, n, 0:8], in_=sall[:, n, :]), reads=[bsall], writes=[bv12])
                    S.op("dve", lambda e, n=n: e.max_index(out=i12u[:, n, 0:8], in_max=v12[:, n, 0:8], in_values=sall[:, n, :]), reads=[bsall, bv12], writes=[bi12])
                    S.op("dve", lambda e, n=n: e.match_replace(out=swork[:, n, :], in_to_replace=v12[:, n, 0:8], in_values=sall[:, n, :], imm_value=-1e30), reads=[bsall, bv12], writes=[bswork])
                    S.op("dve", lambda e, n=n: e.max(out=v12[:, n, 8:16], in_=swork[:, n, :]), reads=[bswork], writes=[bv12])
                    S.op("dve", lambda e, n=n: e.max_index(out=i12u[:, n, 8:16], in_max=v12[:, n, 8:16], in_values=swork[:, n, :]), reads=[bswork, bv12], writes=[bi12])
                S.op("dve", lambda e: e.tensor_copy(out=i12f[:], in_=i12u[:]), reads=[bi12], writes=[bi12])
                v12v = v12[:].rearrange("p (h two) k -> p h two k", two=2); i12v = i12f[:].rearrange("p (h two) k -> p h two k", two=2)
                for h in range(8):
                    cv = cand[:, h, :].rearrange("p (a b) -> p a b", b=16); civ = cidx[:, h, :].rearrange("p (a b) -> p a b", b=16)
                    S.op("dve", lambda e, h=h, cv=cv: e.tensor_tensor(out=cv, in0=v12v[:, h, 0, :].unsqueeze(2).broadcast_to([128, 16, 16]), in1=v12v[:, h, 1, :].unsqueeze(1).broadcast_to([128, 16, 16]), op=ALU.add),
                         reads=[bv12], writes=[bcand])
                    S.op("dve", lambda e, h=h, civ=civ: e.scalar_tensor_tensor(out=civ, in0=i12v[:, h, 0, :].unsqueeze(2).broadcast_to([128, 16, 16]), scalar=128.0, in1=i12v[:, h, 1, :].unsqueeze(1).broadcast_to([128, 16, 16]), op0=ALU.mult, op1=ALU.add),
                         reads=[bi12], writes=[bcidx])
                    S.op("dve", lambda e, h=h: e.max(out=tops[:, h, 0:8], in_=cand[:, h, :]), reads=[bcand], writes=[btops])
                    S.op("dve", lambda e, h=h: e.match_replace(out=cwork[:, h, :], in_to_replace=tops[:, h, 0:8], in_values=cand[:, h, :], imm_value=-1e30), reads=[bcand, btops], writes=[bcwork])
                    S.op("dve", lambda e, h=h: e.max(out=tops[:, h, 8:16], in_=cwork[:, h, :]), reads=[bcwork], writes=[btops])
                    for k8 in range(2):
                        S.op("dve", lambda e, h=h, k8=k8: e.tensor_tensor(out=eq[:], in0=cand[:, h, :].unsqueeze(1).broadcast_to([128, 8, 256]), in1=tops[:, h, k8 * 8:(k8 + 1) * 8].unsqueeze(2).broadcast_to([128, 8, 256]), op=ALU.is_equal),
                             reads=[bcand, btops], writes=[beq])
                        S.op("dve", lambda e, h=h: e.tensor_tensor(out=eq[:], in0=eq[:], in1=cidx[:, h, :].unsqueeze(1).broadcast_to([128, 8, 256]), op=ALU.mult), reads=[beq, bcidx], writes=[beq])
                        S.op("dve", lambda e, h=h, k8=k8: e.tensor_reduce(out=idxf[:, h, k8 * 8:(k8 + 1) * 8], in_=eq[:], axis=AX.X, op=ALU.add), reads=[beq], writes=[bidxf])
                S.op("dve", lambda e: e.tensor_scalar(out=idxf[:], in0=idxf[:], scalar1=float(NEXP - 1), scalar2=0.0, op0=ALU.min, op1=ALU.max), reads=[bidxf], writes=[bidxf])
                S.op("dve", lambda e: e.tensor_copy(out=idxi[:], in_=idxf[:].rearrange("p h k -> p (h k)")), reads=[bidxf], writes=[bidxi])
                S.op("dve", lambda e: e.tensor_reduce(out=gmx[:], in_=tops[:], axis=AX.X, op=ALU.max), reads=[btops], writes=[bgate])
                S.op("dve", lambda e: e.tensor_tensor(out=gate[:], in0=tops[:], in1=gmx[:].unsqueeze(2).broadcast_to([128, 8, 16]), op=ALU.subtract), reads=[btops, bgate], writes=[bgate])
                S.op("act", lambda e: e.activation(out=gate[:], in_=gate[:], func=AF.Exp), reads=[bgate], writes=[bgate])
                S.op("dve", lambda e: e.tensor_reduce(out=gsum[:], in_=gate[:], axis=AX.X, op=ALU.add), reads=[bgate], writes=[bgate])
                S.op("dve", lambda e: e.reciprocal(out=gsum[:], in_=gsum[:]), reads=[bgate], writes=[bgate])
                S.op("dve", lambda e: e.tensor_tensor(out=gate[:], in0=gate[:], in1=gsum[:].unsqueeze(2).broadcast_to([128, 8, 16]), op=ALU.mult), reads=[bgate], writes=[bgate])
                for sl in range(128):
                    i = gi % NG; gi += 1
                    S.dma("pool", lambda e, i=i, sl=sl: e.indirect_dma_start(out=ug[i][:], out_offset=None, in_=pu, in_offset=bass.IndirectOffsetOnAxis(ap=idxi[:, sl:sl + 1], axis=0)),
                          reads=[bidxi], writes=[bug[i]])
                    S.op("dve", lambda e, i=i, sl=sl: e.scalar_tensor_tensor(out=junk[:], in0=ug[i][:], scalar=1.0, in1=h2tok[:], op0=ALU.mult, op1=ALU.mult, accum_out=aact[:, sl:sl + 1]),
                         reads=[bug[i], bh2tok], writes=[bjunk, baact])
                S.op("act", lambda e: e.activation(out=coef[:], in_=aact[:], func=AF.Gelu), reads=[baact], writes=[bcoef])
                S.op("dve", lambda e: e.tensor_tensor(out=coef[:], in0=coef[:], in1=gate[:].rearrange("p h k -> p (h k)"), op=ALU.mult), reads=[bcoef, bgate], writes=[bcoef])
                for sl in range(128):
                    i = gi % NG; gi += 1
                    S.dma("pool", lambda e, i=i, sl=sl: e.indirect_dma_start(out=ug[i][:], out_offset=None, in_=pv, in_offset=bass.IndirectOffsetOnAxis(ap=idxi[:, sl:sl + 1], axis=0)),
                          reads=[bidxi], writes=[bug[i]])
                    if sl == 0:
                        S.op("dve", lambda e, i=i, sl=sl: e.tensor_scalar(out=acc[:], in0=ug[i][:], scalar1=coef[:, sl:sl + 1], scalar2=None, op0=ALU.mult), reads=[bug[i], bcoef], writes=[bacc])
                    else:
                        S.op("dve", lambda e, i=i, sl=sl: e.scalar_tensor_tensor(out=acc[:], in0=ug[i][:], scalar=coef[:, sl:sl + 1], in1=acc[:], op0=ALU.mult, op1=ALU.add), reads=[bug[i], bcoef, bacc], writes=[bacc])
                for half in range(2):
                    for k4 in range(4):
                        kc = half * 4 + k4
                        S.op("pe", lambda e, kc=kc, k4=k4: e.transpose(out=ps_misc[:, k4 * 128:(k4 + 1) * 128], in_=acc[:, kc * 128:(kc + 1) * 128], identity=ident[:]), reads=[bacc, bcst], writes=[bps_misc])
                    for k4 in range(4):
                        kc = half * 4 + k4
                        S.op("dve", lambda e, kc=kc, k4=k4, ts=ts: e.scalar_tensor_tensor(out=x2[:, kc, ts], in0=ps_misc[:, k4 * 128:(k4 + 1) * 128], scalar=g2[:, kc:kc + 1], in1=x1[:, kc, ts], op0=ALU.mult, op1=ALU.add),
                             reads=[bps_misc, bx1, bmod], writes=[bx2])
            if final:
                norm(x2, bx2, fg, None, osb, bosb, bgt)
                src, bsrc = osb, bosb
            else:
                src, bsrc = x2, bx2
            for kc in range(KC):
                S.dma("sp", lambda e, kc=kc, t0=t0, src=src: e.dma_start(out=outT[kc * 128:(kc + 1) * 128, t0:t0 + TB], in_=src[:, kc, :]), reads=[bsrc], writes=[bout])
        S.emit(final_wait_bufs=[bout])
    return nc


def colmajor(v):
    return np.ascontiguousarray(v.reshape(-1, 128).T)


_PROGS = {}


def _prog(key, fn, *args):
    if key not in _PROGS:
        _PROGS[key] = fn(*args)
    return _PROGS[key]


def _run(nc, in_maps):
    res = run_bass_kernel_spmd(nc, in_maps, core_ids=list(range(8)))
    return res.results


def _c(a):
    return np.ascontiguousarray(a, dtype=np.float32)


def kernel(**P):
    P = {k: np.asarray(v) for k, v in P.items()}
    x = P['x']; c = P['c']
    NTOK = 16384
    xT = _c(x.reshape(NTOK, 1024).T)
    U, m01, mneg_sb = sb_consts()
    mneg_fox, sel65 = fox_consts()
    bd, ident, sel2 = rwkv_consts()
    for layer in range(4):
        j = layer // 2
        even = (layer % 2 == 0)
        w_in = P['ev_w_in'][j] if even else P['od_w_in'][j]
        w_out = P['ev_w_out'][j] if even else P['od_w_out'][j]
        NOUT = w_in.shape[1]
        ncA = _prog(('A', NOUT), build_stageA, NOUT)
        adawA = _c(P['ada_w'][layer][:, 0:2048]); adabA = colmajor(P['ada_b'][layer][0:2048]); gainA = colmajor(P['norm_mix'][layer])
        w_in_c = _c(w_in)
        in_maps = [dict(xT=_c(xT[:, k * 2048:(k + 1) * 2048]), cvec=colmajor(c[k // 4]), adaw=adawA, adab=adabA, gain=gainA, w_in=w_in_c) for k in range(8)]
        res = _run(ncA, in_maps)
        projT = np.concatenate([r['projT'] for r in res], axis=1)
        mixT = np.empty((1024, NTOK), np.float32)
        if even:
            ncB = _prog('B0', build_stageB_even)
            in_maps = []
            for k in range(8):
                b = k // 4; hg = k % 4
                pT = projT[:, b * 8192:(b + 1) * 8192]
                in_maps.append(dict(qT=_c(pT[hg * 128:(hg + 1) * 128]).reshape(2, 64, 8192), kT=_c(pT[512 + hg * 128:512 + (hg + 1) * 128]).reshape(2, 64, 8192),
                                    v=_c(pT[1024 + hg * 128:1024 + (hg + 1) * 128].T), xp=_c(pT[1536 + hg * 128:1536 + (hg + 1) * 128].T),
                                    U=U, m01=m01, mneg=mneg_sb, M=np.stack(pool_consts((2, 4, 8, 16)[hg])), pw=_c(P['pool_w'][j][hg]),
                                    psc=_c(P['pool_scale'][j][hg * 128:(hg + 1) * 128].reshape(128, 1))))
            res = _run(ncB, in_maps)
            for k in range(8):
                b = k // 4; hg = k % 4
                mixT[hg * 128:(hg + 1) * 128, b * 8192:(b + 1) * 8192] = res[k]['sbT']
                mixT[512 + hg * 128:512 + (hg + 1) * 128, b * 8192:(b + 1) * 8192] = res[k]['poolT']
        else:
            ncF = _prog('fox', build_fox)
            in_maps = []
            for k in range(8):
                b = k // 4; hg = k % 4
                pT = projT[:, b * 8192:(b + 1) * 8192]
                in_maps.append(dict(qT=_c(pT[hg * 128:(hg + 1) * 128]).reshape(2, 64, 8192), kT=_c(pT[512 + hg * 128:512 + (hg + 1) * 128]).reshape(2, 64, 8192),
                                    v=_c(pT[1024 + hg * 128:1024 + (hg + 1) * 128].T), gT=_c(pT[1536 + hg * 128:1536 + (hg + 1) * 128]).reshape(2, 64, 8192),
                                    fl=_c(pT[2048 + 2 * hg:2048 + 2 * hg + 2]), fb=_c(P['fox_fbias'][j][hg * 2:hg * 2 + 2].reshape(2, 1)),
                                    qn=_c(P['fox_qnorm'][j].reshape(64, 1)), kn=_c(P['fox_knorm'][j].reshape(64, 1)), mneg=mneg_fox, sel=sel65))
            res = _run(ncF, in_maps)
            for k in range(8):
                b = k // 4; hg = k % 4
                mixT[hg * 128:(hg + 1) * 128, b * 8192:(b + 1) * 8192] = res[k]['foxT']
            ncR = _prog('rwkv', build_rwkv)
            in_maps = []
            o = 2056
            mix = P['rwkv_mix'][j]
            for k in range(8):
                b = k // 4; hg = k % 4
                pT = projT[:, b * 8192:(b + 1) * 8192]
                cs = slice(hg * 128, (hg + 1) * 128)
                prm = np.zeros((128, 16), np.float32)
                prm[:, 0] = mix[0:512][cs]; prm[:, 1] = mix[512:1024][cs]; prm[:, 2] = mix[1024:1536][cs]; prm[:, 3] = mix[1664:1792]
                prm[:, 4] = P['rwkv_w0'][j][cs]; prm[:, 5] = P['rwkv_a0'][j][cs]; prm[:, 6] = P['rwkv_kk'][j][cs]; prm[:, 7] = P['rwkv_ka'][j][cs]
                prm[:, 8] = P['rwkv_rk'][j].reshape(-1)[cs]; prm[:, 9] = P['rwkv_ln_w'][j][cs]; prm[:, 10] = P['rwkv_ln_b'][j][cs]
                prm[:64, 11] = mix[1536:1600]; prm[:64, 12] = mix[1600:1664]
                in_maps.append(dict(rT=_c(pT[o + hg * 128:o + (hg + 1) * 128]), kT=_c(pT[o + 512 + hg * 128:o + 512 + (hg + 1) * 128]), vT=_c(pT[o + 1024 + hg * 128:o + 1024 + (hg + 1) * 128]),
                                    xw=_c(pT[o + 1536:o + 1600]), xa=_c(pT[o + 1600:o + 1664]), xg=_c(pT[o + 1664:o + 1792]), prm=prm,
                                    w2=_c(P['rwkv_w2'][j][:, cs]), a2=_c(P['rwkv_a2'][j][:, cs]), g2=_c(P['rwkv_g2'][j][:, cs]), bd=bd, ident=ident, sel2=sel2))
            res = _run(ncR, in_maps)
            for k in range(8):
                b = k // 4; hg = k % 4
                mixT[512 + hg * 128:512 + (hg + 1) * 128, b * 8192:(b + 1) * 8192] = res[k]['rwkvT']
        final = (layer == 3)
        ncC = _prog(('C', final), build_stageC, final)
        adawC = _c(P['ada_w'][layer][:, 2048:6144]); adabC = colmajor(P['ada_b'][layer][2048:6144])
        k12T = _c(np.concatenate([P['peer_k1'][layer].T, P['peer_k2'][layer].T], axis=1))
        gainC = colmajor(P['norm_ffn'][layer]); fgain = colmajor(P['final_norm'])
        w_out_c = _c(w_out); wq_c = _c(P['peer_wq'][layer]); pu = _c(P['peer_u'][layer]); pv = _c(P['peer_v'][layer])
        in_maps = [dict(xT=_c(xT[:, k * 2048:(k + 1) * 2048]), mixT=_c(mixT[:, k * 2048:(k + 1) * 2048]), cvec=colmajor(c[k // 4]), adaw=adawC, adab=adabC,
                        gain=gainC, fgain=fgain, w_out=w_out_c, wq=wq_c, k12T=k12T, ident=ident, pu=pu, pv=pv) for k in range(8)]
        res = _run(ncC, in_maps)
        xT = np.concatenate([r['outT'] for r in res], axis=1)
    return _c(xT.T).reshape(2, 8192, 1024)
```
